# Optimizing a Trainium2 kernel written in Bass

```python
import jax, jax.numpy as jnp
from jax import lax
import numpy as np


D_MODEL = 1024
BATCH = 8
SEQ = 2048
DEPTH = 4

D_MIX = D_MODEL
D_NSA = D_MIX // 2
D_LRU = D_MIX - D_NSA
NSA_HEADS = 8
NSA_HEAD_DIM = D_NSA // NSA_HEADS
NSA_KV_GROUPS = 2
NSA_REP = NSA_HEADS // NSA_KV_GROUPS
NSA_KV_W = NSA_KV_GROUPS * NSA_HEAD_DIM
N_BRANCH = 3
CMP_LEN = 32
CMP_STRIDE = 16
CMP_HIDDEN = 4 * NSA_HEAD_DIM
SEL_BLOCK = 64
SEL_TOP_K = 8
WINDOW = 512
Q_BLOCK = 128
LRU_BLOCKS = 8
LRU_BLOCK_W = D_LRU // LRU_BLOCKS
LRU_CONV_W = 4
LRU_C = 8.0
D_FF = 2816
FFN_CONV_W = 3
NORM_EPS = 1e-6
NEG_INF = -1e30
FORCE_BONUS = 1e4
IN_COLS = D_NSA + 6 * NSA_KV_W + N_BRANCH * NSA_HEADS + 2 * D_LRU

kernel_name = 'hymba_nsa_rglru_convffn_adaln'


def _in_splits():
    widths = [D_NSA] + [NSA_KV_W] * 6 + [N_BRANCH * NSA_HEADS, D_LRU, D_LRU]
    return [int(v) for v in np.cumsum(widths)[:-1]]


def _rmsnorm(x, g):
    xf = x.astype(jnp.float32)
    y = xf * lax.rsqrt(jnp.mean(xf * xf, axis=-1, keepdims=True) + NORM_EPS)
    return y.astype(x.dtype) * g


def _masked_softmax(s, mask):
    s = jnp.where(mask, s.astype(jnp.float32), NEG_INF)
    return jnp.where(mask, jax.nn.softmax(s, axis=-1), 0.0)


def _causal_dwconv(x, w, b):
    width = w.shape[0]
    seq = x.shape[1]
    xp = jnp.pad(x, ((0, 0), (width - 1, 0), (0, 0)))
    y = b
    for k in range(width):
        y = y + xp[:, k:k + seq] * w[k]
    return y


def _split_q(z):
    B, S, _ = z.shape
    return z.reshape(B, S, NSA_KV_GROUPS, NSA_REP, NSA_HEAD_DIM).transpose(0, 2, 3, 1, 4)


def _split_kv(z):
    B, S, _ = z.shape
    return z.reshape(B, S, NSA_KV_GROUPS, NSA_HEAD_DIM).transpose(0, 2, 1, 3)


def _compress(k, pos, w1, b1, w2, b2):
    S = k.shape[2]
    nc = (S - CMP_LEN) // CMP_STRIDE + 1
    idx = np.arange(nc)[:, None] * CMP_STRIDE + np.arange(CMP_LEN)[None, :]
    blocks = k[:, :, idx] + pos
    flat = blocks.reshape(blocks.shape[0], blocks.shape[1], nc, CMP_LEN * NSA_HEAD_DIM)
    return jax.nn.gelu(flat @ w1 + b1) @ w2 + b2


def _nsa_mixer(zq, zkc, zvc, zks, zvs, zkw, zvw, zg, gate_b, cmp_pos, cmp_w1, cmp_b1, cmp_w2, cmp_b2):
    B, S, _ = zq.shape
    G, R, DH = NSA_KV_GROUPS, NSA_REP, NSA_HEAD_DIM
    scale = DH ** -0.5
    t = jnp.arange(S)
    q = _split_q(zq)

    kc = _compress(_split_kv(zkc), cmp_pos[0], cmp_w1[0], cmp_b1[0], cmp_w2[0], cmp_b2[0])
    vc = _compress(_split_kv(zvc), cmp_pos[1], cmp_w1[1], cmp_b1[1], cmp_w2[1], cmp_b2[1])
    nc = kc.shape[2]
    cmp_start = jnp.arange(nc) * CMP_STRIDE
    mask_c = (cmp_start + CMP_LEN - 1)[None, :] <= t[:, None]
    p_cmp = _masked_softmax(jnp.einsum('bgrsd,bgcd->bgrsc', q, kc) * scale, mask_c)
    o_cmp = jnp.einsum('bgrsc,bgcd->bgrsd', p_cmp.astype(vc.dtype), vc)

    nb = S // SEL_BLOCK
    js = jnp.arange(nb) * SEL_BLOCK
    overlap = ((cmp_start[:, None] < js[None, :] + SEL_BLOCK) & (cmp_start[:, None] + CMP_LEN > js[None, :])).astype(jnp.float32)
    imp = jnp.einsum('bgrsc,cj->bgsj', p_cmp, overlap)
    qblk = t // SEL_BLOCK
    jb = jnp.arange(nb)[None, :]
    valid = jb <= qblk[:, None]
    forced = (jb == 0) | (jb == qblk[:, None]) | (jb == qblk[:, None] - 1)
    imp = jnp.where(valid, imp + jnp.where(forced, FORCE_BONUS, 0.0), NEG_INF)
    top_k = min(SEL_TOP_K, nb)
    _, sel = lax.top_k(imp, top_k)

    nq = S // Q_BLOCK
    ks_b = _split_kv(zks).reshape(B, G, nb, SEL_BLOCK, DH)
    vs_b = _split_kv(zvs).reshape(B, G, nb, SEL_BLOCK, DH)
    q_blk = q.reshape(B, G, R, nq, Q_BLOCK, DH).transpose(3, 0, 1, 2, 4, 5)
    sel_blk = sel.reshape(B, G, nq, Q_BLOCK, top_k).transpose(2, 0, 1, 3, 4)
    t_blk = t.reshape(nq, Q_BLOCK)
    bi = jnp.arange(B)[:, None, None, None]
    gi = jnp.arange(G)[None, :, None, None]
    n_sel = top_k * SEL_BLOCK

    def sel_attend(args):
        qb, ib, tb = args
        kg = ks_b[bi, gi, ib].reshape(B, G, Q_BLOCK, n_sel, DH)
        vg = vs_b[bi, gi, ib].reshape(B, G, Q_BLOCK, n_sel, DH)
        kpos = (ib[..., None] * SEL_BLOCK + jnp.arange(SEL_BLOCK)).reshape(B, G, Q_BLOCK, n_sel)
        mask = (kpos <= tb[:, None])[:, :, None]
        p = _masked_softmax(jnp.einsum('bgrqd,bgqnd->bgrqn', qb, kg) * scale, mask)
        return jnp.einsum('bgrqn,bgqnd->bgrqd', p.astype(vg.dtype), vg)

    o_slc = lax.map(sel_attend, (q_blk, sel_blk, t_blk))
    o_slc = o_slc.transpose(1, 2, 3, 0, 4, 5).reshape(B, G, R, S, DH)

    nw = WINDOW // Q_BLOCK + 1
    kp = jnp.pad(_split_kv(zkw), ((0, 0), (0, 0), (WINDOW, 0), (0, 0))).reshape(B, G, nq + nw - 1, Q_BLOCK, DH)
    vp = jnp.pad(_split_kv(zvw), ((0, 0), (0, 0), (WINDOW, 0), (0, 0))).reshape(B, G, nq + nw - 1, Q_BLOCK, DH)
    kband = jnp.concatenate([kp[:, :, i:i + nq] for i in range(nw)], axis=3)
    vband = jnp.concatenate([vp[:, :, i:i + nq] for i in range(nw)], axis=3)
    qw = q.reshape(B, G, R, nq, Q_BLOCK, DH)
    kpos = jnp.arange(nq)[:, None] * Q_BLOCK - WINDOW + jnp.arange(nw * Q_BLOCK)[None, :]
    kpos = kpos[:, None, :]
    tq = t_blk[:, :, None]
    mask_w = (kpos <= tq) & (kpos > tq - WINDOW) & (kpos >= 0)
    p_w = _masked_softmax(jnp.einsum('bgrnqd,bgnkd->bgrnqk', qw, kband) * scale, mask_w)
    o_win = jnp.einsum('bgrnqk,bgnkd->bgrnqd', p_w.astype(vband.dtype), vband).reshape(B, G, R, S, DH)

    g = jax.nn.sigmoid(zg + gate_b).reshape(B, S, G, R, N_BRANCH).transpose(0, 2, 3, 1, 4)
    o = g[..., 0:1] * o_cmp + g[..., 1:2] * o_slc + g[..., 2:3] * o_win
    return o.transpose(0, 3, 1, 2, 4).reshape(B, S, D_NSA)


def _lin_combine(c1, c2):
    a1, b1 = c1
    a2, b2 = c2
    return a1 * a2, a2 * b1 + b2


def _rglru_mixer(zx, zy, conv_w, conv_b, wa, ba, wx, bx, lam):
    B, S, _ = zx.shape
    xc = _causal_dwconv(zx, conv_w, conv_b)
    xh = xc.reshape(B, S, LRU_BLOCKS, LRU_BLOCK_W)
    r = jax.nn.sigmoid(jnp.einsum('bsnc,ncd->bsnd', xh, wa).reshape(B, S, D_LRU) + ba)
    i = jax.nn.sigmoid(jnp.einsum('bsnc,ncd->bsnd', xh, wx).reshape(B, S, D_LRU) + bx)
    log_a = LRU_C * r.astype(jnp.float32) * jax.nn.log_sigmoid(lam.astype(jnp.float32))
    a = jnp.exp(log_a)
    u = jnp.sqrt(-jnp.expm1(2.0 * log_a)) * (i * xc).astype(jnp.float32)
    _, h = lax.associative_scan(_lin_combine, (a, u), axis=1)
    return h.astype(zx.dtype) * jax.nn.gelu(zy)


def _conv_ffn(h, w_gate, w_up, conv_w, conv_b, w_down):
    gate = _causal_dwconv(h @ w_gate, conv_w, conv_b)
    return (jax.nn.silu(gate) * (h @ w_up)) @ w_down


def setup_inputs(seed: int = 0) -> dict:
    key = jax.random.key(seed)
    ks = jax.random.split(key, 32)

    def nrm(k, shape, scale):
        return jax.random.normal(k, shape, jnp.float32) * scale

    L = DEPTH
    lam_u = jax.random.uniform(ks[19], (L, D_LRU), jnp.float32, 0.9, 0.999)
    return {
        'x': nrm(ks[0], (BATCH, SEQ, D_MODEL), 1.0),
        'c': nrm(ks[1], (BATCH, D_MODEL), 1.0),
        'ada_w': nrm(ks[2], (L, D_MODEL, 6 * D_MODEL), 0.5 * D_MODEL ** -0.5),
        'ada_b': nrm(ks[3], (L, 6 * D_MODEL), 0.02),
        'mix_norm_g': 1.0 + nrm(ks[4], (L, D_MODEL), 0.02),
        'ffn_norm_g': 1.0 + nrm(ks[5], (L, D_MODEL), 0.02),
        'w_in': nrm(ks[6], (L, D_MODEL, IN_COLS), D_MODEL ** -0.5),
        'nsa_gate_b': nrm(ks[7], (L, N_BRANCH * NSA_HEADS), 0.1),
        'cmp_pos': nrm(ks[8], (L, 2, CMP_LEN, NSA_HEAD_DIM), 0.02),
        'cmp_w1': nrm(ks[9], (L, 2, CMP_LEN * NSA_HEAD_DIM, CMP_HIDDEN), (CMP_LEN * NSA_HEAD_DIM) ** -0.5),
        'cmp_b1': nrm(ks[10], (L, 2, CMP_HIDDEN), 0.01),
        'cmp_w2': nrm(ks[11], (L, 2, CMP_HIDDEN, NSA_HEAD_DIM), CMP_HIDDEN ** -0.5),
        'cmp_b2': nrm(ks[12], (L, 2, NSA_HEAD_DIM), 0.01),
        'lru_conv_w': nrm(ks[13], (L, LRU_CONV_W, D_LRU), LRU_CONV_W ** -0.5),
        'lru_conv_b': nrm(ks[14], (L, D_LRU), 0.01),
        'lru_wa': nrm(ks[15], (L, LRU_BLOCKS, LRU_BLOCK_W, LRU_BLOCK_W), LRU_BLOCK_W ** -0.5),
        'lru_ba': nrm(ks[16], (L, D_LRU), 0.01),
        'lru_wx': nrm(ks[17], (L, LRU_BLOCKS, LRU_BLOCK_W, LRU_BLOCK_W), LRU_BLOCK_W ** -0.5),
        'lru_bx': nrm(ks[18], (L, D_LRU), 0.01),
        'lru_lambda': jnp.log(lam_u) - jnp.log1p(-lam_u),
        'nsa_out_norm_g': 1.0 + nrm(ks[20], (L, D_NSA), 0.02),
        'lru_out_norm_g': 1.0 + nrm(ks[21], (L, D_LRU), 0.02),
        'w_out': nrm(ks[22], (L, D_MIX, D_MODEL), D_MIX ** -0.5),
        'ffn_w_gate': nrm(ks[23], (L, D_MODEL, D_FF), D_MODEL ** -0.5),
        'ffn_w_up': nrm(ks[24], (L, D_MODEL, D_FF), D_MODEL ** -0.5),
        'ffn_conv_w': nrm(ks[25], (L, FFN_CONV_W, D_FF), FFN_CONV_W ** -0.5),
        'ffn_conv_b': nrm(ks[26], (L, D_FF), 0.01),
        'ffn_w_down': nrm(ks[27], (L, D_FF, D_MODEL), D_FF ** -0.5),
        'final_norm_g': 1.0 + nrm(ks[28], (D_MODEL,), 0.02),
    }


def reference(x, c, ada_w, ada_b, mix_norm_g, ffn_norm_g, w_in, nsa_gate_b, cmp_pos, cmp_w1, cmp_b1, cmp_w2, cmp_b2, lru_conv_w, lru_conv_b, lru_wa, lru_ba, lru_wx, lru_bx, lru_lambda, nsa_out_norm_g, lru_out_norm_g, w_out, ffn_w_gate, ffn_w_up, ffn_conv_w, ffn_conv_b, ffn_w_down, final_norm_g):
    splits = _in_splits()
    c_act = jax.nn.silu(c)
    for l in range(DEPTH):
        mod = (c_act @ ada_w[l] + ada_b[l])[:, None, :]
        sh1, sc1, g1, sh2, sc2, g2 = jnp.split(mod, 6, axis=-1)

        h = _rmsnorm(x, mix_norm_g[l]) * (1.0 + sc1) + sh1
        z = h @ w_in[l]
        zq, zkc, zvc, zks, zvs, zkw, zvw, zg, zx, zy = jnp.split(z, splits, axis=-1)
        o_nsa = _nsa_mixer(zq, zkc, zvc, zks, zvs, zkw, zvw, zg, nsa_gate_b[l], cmp_pos[l], cmp_w1[l], cmp_b1[l], cmp_w2[l], cmp_b2[l])
        o_lru = _rglru_mixer(zx, zy, lru_conv_w[l], lru_conv_b[l], lru_wa[l], lru_ba[l], lru_wx[l], lru_bx[l], lru_lambda[l])
        mixed = jnp.concatenate([_rmsnorm(o_nsa, nsa_out_norm_g[l]), _rmsnorm(o_lru, lru_out_norm_g[l])], axis=-1)
        x = x + g1 * (mixed @ w_out[l])

        h = _rmsnorm(x, ffn_norm_g[l]) * (1.0 + sc2) + sh2
        x = x + g2 * _conv_ffn(h, ffn_w_gate[l], ffn_w_up[l], ffn_conv_w[l], ffn_conv_b[l], ffn_w_down[l])
    return _rmsnorm(x, final_norm_g)
```

```python
import numpy as np
import ml_dtypes
import concourse.bass as bass
import concourse.mybir as mybir
from concourse.bass_utils import run_bass_kernel_spmd

F32 = mybir.dt.float32
BF16 = mybir.dt.bfloat16
AF = mybir.ActivationFunctionType
ALU = mybir.AluOpType

L = 4
D = 1024
S = 2048
NT = 16
DFF = 2816
NM = 22
INC = 2328
NEG = -30000.0
FFN_GROUPS = [(0, 5), (5, 5), (10, 4), (14, 4), (18, 4)]


class _IMap:
    def __init__(self):
        self.recs = []

    def access(self, lo, hi, tok, is_write, deps):
        out = []
        for rec in self.recs:
            rlo, rhi, w, rd = rec
            if rhi <= lo or rlo >= hi:
                out.append(rec)
                continue
            if w is not None:
                deps.append(w)
            if is_write:
                deps.extend(rd.values())
                if rlo < lo:
                    out.append([rlo, lo, w, dict(rd)])
                if rhi > hi:
                    out.append([hi, rhi, w, dict(rd)])
            else:
                if rlo < lo:
                    out.append([rlo, lo, w, dict(rd)])
                if rhi > hi:
                    out.append([hi, rhi, w, dict(rd)])
                nrd = dict(rd)
                k = id(tok[0])
                if k not in nrd or nrd[k][1] < tok[1]:
                    nrd[k] = tok
                out.append([max(lo, rlo), min(hi, rhi), w, nrd])
        if is_write:
            out.append([lo, hi, tok, {}])
        else:
            covered = sorted((r[0], r[1]) for r in out if not (r[1] <= lo or r[0] >= hi))
            cur = lo
            for a, b in covered:
                if a > cur:
                    out.append([cur, a, None, {id(tok[0]): tok}])
                cur = max(cur, b)
            if cur < hi:
                out.append([cur, hi, None, {id(tok[0]): tok}])
        self.recs = out


def _esize(dt):
    if dt == F32:
        return 4
    if dt == BF16:
        return 2
    return mybir.dt.size(dt)


def ap_intervals(ap):
    t = ap.tensor
    es = _esize(ap.dtype)
    dims = [list(d) for d in ap.ap]
    space = str(ap.space).upper()
    if 'DRAM' in space or 'HBM' in space:
        off = ap.offset
        fd = dims
        key = 'D:' + t.name
    else:
        pstride = dims[0][0]
        off = ap.offset % pstride if pstride > 0 else ap.offset
        fd = dims[1:]
        key = ('P:' if 'PSUM' in space else 'S:') + t.name
    fd = [d for d in fd if d[1] > 1 and d[0] != 0]
    fd.sort(key=lambda d: -abs(d[0]))
    ivs = [(off, off + 1)]
    for step, cnt in reversed(fd):
        step = abs(step)
        ext = ivs[-1][1] - ivs[0][0]
        if step <= ext or len(ivs) * cnt > 40:
            ivs = [(ivs[0][0], ivs[-1][1] + (cnt - 1) * step)]
        else:
            n = []
            for i in range(cnt):
                for (a, b) in ivs:
                    n.append((a + i * step, b + i * step))
            ivs = n
    return key, [(a * es, b * es) for a, b in ivs]


class FW:
    ENGS = ['pe', 'act', 'dve', 'pool', 'sp']
    EPOCH = 30000

    def __init__(self, nc, n_dma_sems=12):
        self.nc = nc
        self.prog = {e: [] for e in self.ENGS}
        self.sem = {e: nc.alloc_semaphore(name='c_' + e) for e in self.ENGS}
        self.cnt = {e: 0 for e in self.ENGS}
        self.tot = {e: 0 for e in self.ENGS}
        self.seen = {e: {} for e in self.ENGS}
        self.maps = {}
        self.dsem = {}
        for q in ['sp', 'pool']:
            self.dsem[q] = [[nc.alloc_semaphore(name=f'd_{q}{i}'), 0] for i in range(n_dma_sems)]
        self.drr = {q: 0 for q in self.dsem}
        self.out_toks = []
        self.last_tok = {}

    def _deps(self, tok, reads, writes):
        deps = []
        for ap in reads:
            key, ivs = ap_intervals(ap)
            m = self.maps.setdefault(key, _IMap())
            for lo, hi in ivs:
                m.access(lo, hi, tok, False, deps)
        for ap in writes:
            key, ivs = ap_intervals(ap)
            m = self.maps.setdefault(key, _IMap())
            for lo, hi in ivs:
                m.access(lo, hi, tok, True, deps)
        return deps

    def _waits(self, e, deps, tok=None, skip_self=False):
        need = {}
        for d in deps:
            if tok is not None and d[0] is tok[0] and d[1] >= tok[1]:
                continue
            if skip_self and tok is not None and d[0] is tok[0]:
                continue
            k = id(d[0])
            if k not in need or need[k][1] < d[1]:
                need[k] = d
        w = []
        for k, d in need.items():
            if self.seen[e].get(k, 0) >= d[1]:
                continue
            self.seen[e][k] = d[1]
            w.append((d[0], d[1]))
        return w

    def op(self, e, fn, reads=(), writes=()):
        if self.cnt[e] >= self.EPOCH:
            self.last_tok[e] = (self.sem[e], self.cnt[e])
            self.sem[e] = self.nc.alloc_semaphore(name=f'c_{e}_{self.tot[e]}')
            self.cnt[e] = 0
        self.cnt[e] += 1
        self.tot[e] += 1
        tok = (self.sem[e], self.cnt[e])
        deps = self._deps(tok, reads, writes)
        w = self._waits(e, deps, tok, skip_self=(e == 'pe'))
        self.prog[e].append((w, fn, (self.sem[e], 1)))
        return tok

    def dma(self, q, out, in_, is_output=False, **kw):
        slot = self.dsem[q][self.drr[q] % len(self.dsem[q])]
        self.drr[q] += 1
        sem = slot[0]
        prev = slot[1]
        slot[1] += 16
        tok = (sem, slot[1])
        deps = self._deps(tok, [in_], [out])
        if prev > 0:
            deps.append((sem, prev))
        w = self._waits(q, deps, tok)
        self.prog[q].append((w, lambda eng: eng.dma_start(out=out, in_=in_, **kw), (sem, 16)))
        if is_output:
            self.out_toks.append(tok)
        return tok

    def finish(self, e='sp'):
        deps = list(self.out_toks)
        for q in self.dsem:
            for sem, v in self.dsem[q]:
                if v > 0:
                    deps.append((sem, v))
        for x in self.ENGS:
            if x != e and self.cnt[x] > 0:
                deps.append((self.sem[x], self.cnt[x]))
        w = self._waits(e, deps)
        self.prog[e].append((w, None, None))

    def emit(self):
        nc = self.nc
        with nc.Block() as block:
            def mk(e):
                def body(eng):
                    for w, fn, inc in self.prog[e]:
                        for sem, val in w:
                            eng.wait_ge(sem, val)
                        if fn is not None:
                            ins = fn(eng)
                            ins.then_inc(inc[0], inc[1])
                return body
            block.tensor(mk('pe'))
            block.scalar(mk('act'))
            block.vector(mk('dve'))
            block.gpsimd(mk('pool'))
            block.sync(mk('sp'))


class Arena:
    def __init__(self, nc, nbytes):
        self.t = nc.alloc_sbuf_tensor("arena", [128, nbytes // 2], BF16)
        self.top = 0
        self.cap = nbytes
        self.peak = 0

    def alloc(self, free_shape, dtype):
        n = 1
        for s in free_shape:
            n *= s
        nb = n * _esize(dtype)
        nb_al = (nb + 63) // 64 * 64
        off = self.top
        self.top += nb_al
        self.peak = max(self.peak, self.top)
        assert self.top <= self.cap, f"arena overflow {self.top} > {self.cap}"
        a = self.t[:, off // 2: off // 2 + nb // 2]
        if dtype != BF16:
            a = a.bitcast(dtype)
        if len(free_shape) == 2:
            a = a.rearrange("p (a b) -> p a b", a=free_shape[0])
        elif len(free_shape) == 3:
            a = a.rearrange("p (a b c) -> p a b c", a=free_shape[0], b=free_shape[1])
        return a


def make_consts():
    bf = ml_dtypes.bfloat16
    c = {}
    c['identb'] = np.eye(128, dtype=np.float32).astype(bf)
    c['identf'] = np.eye(128, dtype=np.float32)
    kk = np.arange(128)[:, None]
    tt = np.arange(128)[None, :]
    c['tri1'] = np.where(kk <= tt, 0.0, NEG).astype(bf)
    c['tri2'] = np.where(kk > tt, 0.0, NEG).astype(bf)
    cc = np.arange(128)[:, None]
    t = np.arange(S)[None, :]
    c['cmpb'] = np.where((16 * cc + 31 <= t) & (cc < 127), 0.0, NEG).astype(bf)
    e64 = np.zeros((128, S), np.float32)
    E = (np.arange(S)[None, :] // 64 == np.arange(32)[:, None]).astype(np.float32)
    e64[0:32] = E
    e64[64:96] = E
    c['emat'] = e64.astype(bf)
    t = np.arange(S)
    qblk = t // 64
    j = np.arange(32)
    valid = j[None, :] <= qblk[:, None]
    forced = (j[None, :] == 0) | (j[None, :] == qblk[:, None]) | (j[None, :] == qblk[:, None] - 1)
    cbias = np.where(valid, np.where(forced, 1e4, 0.0), -1e30).astype(np.float32)
    validb = np.where(valid, 0.0, NEG).astype(np.float32)
    c['cbias'] = np.ascontiguousarray(cbias.reshape(NT, 128, 32).transpose(1, 0, 2))
    c['validb'] = np.ascontiguousarray(validb.reshape(NT, 128, 32).transpose(1, 0, 2))
    cs = np.arange(128) * 16
    js = np.arange(32) * 64
    ov = ((cs[:, None] < js[None, :] + 64) & (cs[:, None] + 32 > js[None, :])).astype(np.float32)
    ov[127] = 0
    vca = np.zeros((128, 33), np.float32)
    vca[:, 0] = 1.0
    vca[:, 1:] = ov
    c['vcaux'] = vca.astype(bf)
    return c


WEIGHT_SPECS = [
    ('ada_w', [L, D, 6 * D]), ('ada_b', [L, 6 * D]), ('mix_norm_g', [L, D]), ('ffn_norm_g', [L, D]),
    ('w_in', [L, D, INC]), ('nsa_gate_b', [L, 24]), ('cmp_pos', [L, 2, 32, 64]), ('cmp_w1', [L, 2, 2048, 256]),
    ('cmp_b1', [L, 2, 256]), ('cmp_w2', [L, 2, 256, 64]), ('cmp_b2', [L, 2, 64]), ('lru_conv_w', [L, 4, 512]),
    ('lru_conv_b', [L, 512]), ('lru_wa', [L, 8, 64, 64]), ('lru_ba', [L, 512]), ('lru_wx', [L, 8, 64, 64]),
    ('lru_bx', [L, 512]), ('lru_lambda', [L, 512]), ('nsa_out_norm_g', [L, 512]), ('lru_out_norm_g', [L, 512]),
    ('w_out', [L, D, D]), ('ffn_w_gate', [L, D, DFF]), ('ffn_w_up', [L, D, DFF]), ('ffn_conv_w', [L, 3, DFF]),
    ('ffn_conv_b', [L, DFF]), ('ffn_w_down', [L, DFF, D]), ('final_norm_g', [D]),
]
CONST_SPECS = [('identb', [128, 128], BF16), ('identf', [128, 128], F32), ('tri1', [128, 128], BF16),
               ('tri2', [128, 128], BF16), ('cmpb', [128, S], BF16), ('emat', [128, S], BF16),
               ('cbias', [128, NT, 32], F32), ('validb', [128, NT, 32], F32), ('vcaux', [128, 33], BF16)]


def build_program(n_layers=L, stop=None, dbg=False):
    nc = bass.Bass("TRN2", target_bir_lowering=False)
    fw = FW(nc)
    W = {}
    x_d = nc.dram_tensor("x", [S, D], F32, kind="ExternalInput").ap()
    c_d = nc.dram_tensor("c", [8, 128], F32, kind="ExternalInput").ap()
    for nm, shp in WEIGHT_SPECS:
        shp = list(shp)
        if len(shp) > 1 or nm != 'final_norm_g':
            shp[0] = n_layers
        W[nm] = nc.dram_tensor(nm, shp, F32, kind="ExternalInput").ap()
    CD = {}
    for nm, shp, dt in CONST_SPECS:
        CD[nm] = nc.dram_tensor(nm, shp, dt, kind="ExternalInput").ap()
    out_d = nc.dram_tensor("out", [S, D], F32, kind="ExternalOutput").ap()
    olt_d = nc.dram_tensor("olt_scr", [128, 4, S], BF16, kind="Internal").ap()
    dbg_outs = {}

    ar = Arena(nc, 207 * 1024)
    PS = [nc.alloc_psum_tensor(f"bank{i}", [128, 512], F32) for i in range(8)]
    PSB = [p[:, :].bitcast(BF16) for p in PS]

    def mm(out, lhsT, rhs, start, stop):
        fw.op('pe', lambda e: e.matmul(out, lhsT=lhsT, rhs=rhs, start=start, stop=stop, skip_group_check=True),
              reads=[lhsT, rhs], writes=[out])

    def tr(out, in_, ident):
        fw.op('pe', lambda e: e.transpose(out=out, in_=in_, identity=ident), reads=[in_, ident], writes=[out])

    def act(out, in_, func, bias=None, scale=None, accum=None):
        kw = {}
        rd = [in_]
        wr = [out]
        if bias is not None:
            kw['bias'] = bias
            if not isinstance(bias, float):
                rd.append(bias)
        if scale is not None:
            kw['scale'] = scale
            if not isinstance(scale, float):
                rd.append(scale)
        if accum is not None:
            kw['accum_out'] = accum
            wr.append(accum)
        fw.op('act', lambda e: e.activation(out=out, in_=in_, func=func, **kw), reads=rd, writes=wr)

    def tt(eng, out, in0, in1, op):
        fw.op(eng, lambda e: e.tensor_tensor(out=out, in0=in0, in1=in1, op=op), reads=[in0, in1], writes=[out])

    def ts(eng, out, in0, s1, op0, s2=None, op1=None):
        rd = [in0]
        if not isinstance(s1, float):
            rd.append(s1)
        if s2 is not None and not isinstance(s2, float):
            rd.append(s2)
        if op1 is None:
            fw.op(eng, lambda e: e.tensor_scalar(out=out, in0=in0, scalar1=s1, scalar2=None, op0=op0), reads=rd, writes=[out])
        else:
            fw.op(eng, lambda e: e.tensor_scalar(out=out, in0=in0, scalar1=s1, scalar2=s2, op0=op0, op1=op1), reads=rd, writes=[out])

    def stt(out, in0, scalar, in1, op0, op1):
        rd = [in0, in1]
        if not isinstance(scalar, float):
            rd.append(scalar)
        fw.op('dve', lambda e: e.scalar_tensor_tensor(out=out, in0=in0, scalar=scalar, in1=in1, op0=op0, op1=op1), reads=rd, writes=[out])

    def cp(eng, out, in_):
        if eng == 'act':
            fw.op('act', lambda e: e.copy(out=out, in_=in_), reads=[in_], writes=[out])
        else:
            fw.op(eng, lambda e: e.tensor_copy(out=out, in_=in_), reads=[in_], writes=[out])

    def memset(eng, out, val):
        fw.op(eng, lambda e: e.memset(out, val), writes=[out])

    ev_ctr = [0]

    def evac(out, in_):
        ev_ctr[0] += 1
        cp('act' if ev_ctr[0] % 2 else 'dve', out, in_)

    def dump(name, ap, dt=F32):
        if not dbg:
            return
        shp = list(ap.shape)
        d = nc.dram_tensor("dbg_" + name, shp, dt, kind="ExternalOutput").ap()
        dbg_outs[name] = shp
        fw.dma('sp', d, ap, is_output=True)

    X = ar.alloc([NT, D], F32)
    identb = ar.alloc([128], BF16)
    identf = ar.alloc([128], F32)
    tri1 = ar.alloc([128], BF16)
    tri2 = ar.alloc([128], BF16)
    cmpb = ar.alloc([S], BF16)
    emat = ar.alloc([S], BF16)
    cbias = ar.alloc([NT, 32], F32)
    validb = ar.alloc([NT, 32], F32)
    vcaux = ar.alloc([33], BF16)
    onesb = ar.alloc([128], BF16)
    epsc = ar.alloc([1], F32)
    CREP = ar.alloc([8, 128], BF16)
    ROWS = ar.alloc([128], F32)
    COLA = ar.alloc([64], F32)
    COLB = ar.alloc([96], F32)
    CL = ar.alloc([4], F32)
    WABD = ar.alloc([4, 128], BF16)
    WXBD = ar.alloc([4, 128], BF16)
    SS = ar.alloc([NT], F32)
    RS = ar.alloc([NT], F32)
    SSL = ar.alloc([NT], F32)
    SSN = ar.alloc([NT], F32)
    RSL = ar.alloc([NT], F32)
    RSN = ar.alloc([NT], F32)
    GATES = ar.alloc([NT, 24], F32)
    GBB = ar.alloc([24], F32)
    HLAST = ar.alloc([1], F32)
    base_top = ar.top

    for i in range(4):
        fw.dma('sp', X[:, 4 * i:4 * i + 4, :], x_d.rearrange("(t p) d -> p t d", p=128)[:, 4 * i:4 * i + 4, :])
    for nm, ap_ in [('identb', identb), ('identf', identf), ('tri1', tri1), ('tri2', tri2), ('cmpb', cmpb),
                    ('emat', emat), ('cbias', cbias), ('validb', validb), ('vcaux', vcaux)]:
        fw.dma('sp', ap_, CD[nm])
    memset('dve', onesb, 1.0)
    memset('dve', epsc, 1e-6)
    memset('pool', WABD, 0.0)
    memset('pool', WXBD, 0.0)
    fw.dma('sp', ROWS[0:8, :], c_d)
    tr(PS[0][:, 0:8], ROWS[0:8, :], identf[0:8, 0:8])
    ccol = ar.alloc([8], F32)
    ccolb = ar.alloc([8], BF16)
    act(ccol, PS[0][:, 0:8], AF.Silu)
    cp('dve', ccolb, ccol)
    for kc in range(8):
        cp('dve', CREP[:, kc, :], ccolb[:, kc:kc + 1].to_broadcast([128, 128]))
    setup_top = ar.top

    def rstd_from_ss(ss_ap, out_ap, n):
        act(out_ap, ss_ap, AF.Sqrt, bias=epsc[:, 0:1], scale=1.0 / n)
        fw.op('dve', lambda e: e.reciprocal(out=out_ap, in_=out_ap), reads=[out_ap], writes=[out_ap])

    def mod_half(l, half, MOD, gnorm_b):
        mark = ar.top
        ADAW = [ar.alloc([8, 512], BF16) for _ in range(2)]
        ADAB = [ar.alloc([512], BF16) for _ in range(2)]
        for ch in range(6):
            col0 = (half * 6 + ch) * 512
            aw = ADAW[ch % 2]
            ab = ADAB[ch % 2]
            fw.dma('pool', aw, W['ada_w'][l].rearrange("(kc p) n -> p kc n", p=128)[:, :, col0:col0 + 512])
            fw.dma('pool', ab[0:1, :], W['ada_b'][l:l + 1, col0:col0 + 512])
            bank = PS[ch % 2]
            for kc in range(8):
                mm(bank[:, :], CREP[:, kc, :], aw[:, kc, :], kc == 0, False)
            mm(bank[:, :], onesb[0:1, :], ab[0:1, :], False, True)
            which = ch // 2
            dst = MOD[:, which, (ch % 2) * 512:(ch % 2) * 512 + 512]
            if which == 1:
                stt(dst, bank[:, :], 1.0, gnorm_b[:, (ch % 2) * 512:(ch % 2) * 512 + 512], ALU.add, ALU.mult)
            else:
                cp('act', dst, bank[:, :])
        ar.top = mark

    def norm_to_xnt(XNT, gs_b, sh_b):
        mark = ar.top
        junk = ar.alloc([D], BF16)
        TMP = [ar.alloc([D], F32) for _ in range(2)]
        HB = [ar.alloc([D], BF16) for _ in range(2)]
        for t in range(NT):
            act(junk, X[:, t, :], AF.Square, accum=SS[:, t:t + 1])
        rstd_from_ss(SS, RS, D)
        for t in range(NT):
            tmp = TMP[t % 2]
            hb = HB[t % 2]
            stt(tmp, X[:, t, :], RS[:, t:t + 1], gs_b, ALU.mult, ALU.mult)
            tt('pool', hb, tmp, sh_b, ALU.add)
            pb = PSB[6 + (t % 2)].rearrange("p (a b) -> p a b", a=8)
            for kc in range(8):
                tr(pb[:, kc, :], hb[:, kc * 128:(kc + 1) * 128], identb)
            evac(XNT[:, :, t * 128:(t + 1) * 128], pb)
        ar.top = mark

    for l in range(n_layers):
        ar.top = setup_top
        fw.dma('sp', ROWS[0:16, :], W['lru_conv_w'][l].rearrange("k (ci p) -> (k ci) p", p=128))
        for i, nm in enumerate(['lru_conv_b', 'lru_ba', 'lru_bx', 'lru_lambda', 'lru_out_norm_g', 'nsa_out_norm_g']):
            fw.dma('sp', ROWS[16 + 4 * i:20 + 4 * i, :], W[nm][l].rearrange("(ci p) -> ci p", p=128))
        fw.dma('sp', ROWS[40:44, :], W['cmp_b1'][l].rearrange("kv (jc p) -> (kv jc) p", p=128))
        fw.dma('sp', ROWS[44:45, 0:64], W['cmp_b2'][l, 0:1, :])
        fw.dma('sp', ROWS[44:45, 64:128], W['cmp_b2'][l, 0:1, :])
        tr(PS[0][:, 0:45], ROWS[0:45, :], identf[0:45, 0:45])
        cp('act', COLA[:, 0:45], PS[0][:, 0:45])
        act(CL, COLA[:, 28:32], AF.Exp, scale=-1.0)
        act(CL, CL, AF.Ln, bias=1.0)
        ts('dve', CL, CL, -8.0, ALU.mult)
        for n in range(8):
            ci, hh = n // 2, n % 2
            fw.dma('pool', WABD[hh * 64:hh * 64 + 64, ci, hh * 64:hh * 64 + 64], W['lru_wa'][l, n])
            fw.dma('pool', WXBD[hh * 64:hh * 64 + 64, ci, hh * 64:hh * 64 + 64], W['lru_wx'][l, n])
        fw.dma('sp', GBB, W['nsa_gate_b'][l].partition_broadcast(128))

        mix_top = ar.top
        MOD = ar.alloc([3, D], F32)
        xnt_off = ar.top
        XNT = ar.alloc([8, S], BF16)
        mark_ = ar.top
        GN = ar.alloc([D], F32)
        fw.dma('sp', GN, W['mix_norm_g'][l].partition_broadcast(128))
        mod_half(l, 0, MOD, GN)
        ar.top = mark_
        norm_to_xnt(XNT, MOD[:, 1, :], MOD[:, 0, :])
        if stop == 'norm1':
            dump('xnt', XNT, BF16)
            dump('mod', MOD)
            break

        lru_top = ar.top
        OLT = ar.alloc([4, S], BF16)
        WL = ar.alloc([8, 1024], BF16)
        fw.dma('pool', WL[:, :, 0:512], W['w_in'][l].rearrange("(kc p) n -> p kc n", p=128)[:, :, 1304:1816])
        fw.dma('pool', WL[:, :, 512:1024], W['w_in'][l].rearrange("(kc p) n -> p kc n", p=128)[:, :, 1816:2328])
        TL = 1024
        ZX = ar.alloc([3 + TL], F32)
        XC = ar.alloc([TL], F32)
        GY = ar.alloc([TL], F32)
        A_ = ar.alloc([TL], F32)
        I_ = ar.alloc([TL], F32)
        T_ = ar.alloc([TL], F32)
        XCB = ar.alloc([TL], BF16)
        OSQ = ar.alloc([TL], BF16)
        for ci in range(4):
            cw = [COLA[:, k * 4 + ci:k * 4 + ci + 1] for k in range(4)]
            cb = COLA[:, 16 + ci:17 + ci]
            ba = COLA[:, 20 + ci:21 + ci]
            bx = COLA[:, 24 + ci:25 + ci]
            gl = COLA[:, 32 + ci:33 + ci]
            for hf in range(2):
                if hf == 0:
                    memset('dve', ZX[:, 0:3], 0.0)
                else:
                    cp('dve', ZX[:, 0:3], ZX[:, TL:TL + 3])
                for tci in range(2):
                    tok0 = hf * TL + tci * 512
                    for kc in range(8):
                        mm(PS[tci][:, :], WL[:, kc, ci * 128:(ci + 1) * 128], XNT[:, kc, tok0:tok0 + 512], kc == 0, kc == 7)
                    for kc in range(8):
                        mm(PS[2 + tci][:, :], WL[:, kc, 512 + ci * 128:512 + (ci + 1) * 128], XNT[:, kc, tok0:tok0 + 512], kc == 0, kc == 7)
                    cp('dve', ZX[:, 3 + tci * 512:3 + tci * 512 + 512], PS[tci][:, :])
                    act(GY[:, tci * 512:tci * 512 + 512], PS[2 + tci][:, :], AF.Gelu_apprx_tanh)
                act(XC, ZX[:, 3:3 + TL], AF.Identity, bias=cb, scale=cw[3])
                for k in range(3):
                    stt(XC, ZX[:, k:k + TL], cw[k], XC, ALU.mult, ALU.add)
                cp('pool', XCB, XC)
                for tci in range(2):
                    mm(PS[4 + tci][:, :], WABD[:, ci, :], XCB[:, tci * 512:tci * 512 + 512], True, True)
                    mm(PS[6 + tci][:, :], WXBD[:, ci, :], XCB[:, tci * 512:tci * 512 + 512], True, True)
                for tci in range(2):
                    act(A_[:, tci * 512:tci * 512 + 512], PS[4 + tci][:, :], AF.Sigmoid, bias=ba)
                    act(I_[:, tci * 512:tci * 512 + 512], PS[6 + tci][:, :], AF.Sigmoid, bias=bx)
                act(A_, A_, AF.Exp, scale=CL[:, ci:ci + 1])
                tt('dve', T_, A_, A_, ALU.mult)
                act(T_, T_, AF.Sqrt, bias=1.0, scale=-1.0)
                tt('dve', I_, I_, XC, ALU.mult)
                tt('dve', I_, I_, T_, ALU.mult)
                init = 0.0 if hf == 0 else HLAST[:, 0:1]
                rd = [A_, I_] + ([] if hf == 0 else [HLAST])
                fw.op('dve', lambda e, init=init: e.tensor_tensor_scan(out=T_, data0=A_, data1=I_, initial=init, op0=ALU.mult, op1=ALU.add),
                      reads=rd, writes=[T_])
                cp('dve', HLAST, T_[:, TL - 1:TL])
                tt('dve', GY, T_, GY, ALU.mult)
                tt('pool', OSQ, GY, GY, ALU.mult)
                act(OLT[:, ci, hf * TL:(hf + 1) * TL], GY, AF.Identity, scale=gl)
                for tl in range(8):
                    mm(PS[0][:, tl:tl + 1], OSQ[:, tl * 128:(tl + 1) * 128], onesb[:, 0:1], tl == 0, tl == 7)
                if ci == 0:
                    cp('dve', SSL[:, hf * 8:hf * 8 + 8], PS[0][:, 0:8])
                else:
                    tt('dve', SSL[:, hf * 8:hf * 8 + 8], SSL[:, hf * 8:hf * 8 + 8], PS[0][:, 0:8], ALU.add)
        rstd_from_ss(SSL, RSL, 512)
        if stop == 'lru':
            dump('olt', OLT, BF16)
            dump('rsl', RSL)
            break
        fw.dma('sp', olt_d, OLT)
        ar.top = lru_top

        KCT = ar.alloc([2, 128], BF16)
        VCA = ar.alloc([2, 97], BF16)
        for g in range(2):
            cp('pool', VCA[:, g, 64:97], vcaux)
        cmp_top = ar.top
        WB = [ar.alloc([8, 512], BF16) for _ in range(1)]
        win_v = W['w_in'][l].rearrange("(kc p) n -> p kc n", p=128)

        def load_dup(wb, base_col):
            pass

        wb = WB[0]
        for j, c0 in enumerate([512, 576, 640, 704]):
            fw.dma('pool', wb[:, :, j * 128:j * 128 + 64], win_v[:, :, c0:c0 + 64])
            fw.dma('pool', wb[:, :, j * 128 + 64:j * 128 + 128], win_v[:, :, c0:c0 + 64])
        W1 = ar.alloc([2, 16, 256], BF16)
        for kv in range(2):
            fw.dma('pool', W1[:, kv, :, :], W['cmp_w1'][l, kv].rearrange("(i p) j -> p i j", p=128))
        W2K = ar.alloc([2, 128], BF16)
        W2V = ar.alloc([2, 64], BF16)
        fw.dma('pool', W2K[:, :, 0:64], W['cmp_w2'][l, 0].rearrange("(jc p) d -> p jc d", p=128))
        fw.dma('pool', W2K[:, :, 64:128], W['cmp_w2'][l, 0].rearrange("(jc p) d -> p jc d", p=128))
        fw.dma('pool', W2V, W['cmp_w2'][l, 1].rearrange("(jc p) d -> p jc d", p=128))
        B2V = ar.alloc([64], BF16)
        fw.dma('pool', B2V[0:1, :], W['cmp_b2'][l, 1:2, :])
        POSC = ar.alloc([2, 16], BF16)
        PB1 = ar.alloc([4], F32)
        for kv in range(2):
            fw.dma('sp', ROWS[64 + kv * 16:64 + kv * 16 + 16, :], W['cmp_pos'][l, kv].rearrange("(i a) d -> i (a d)", a=2))
        tr(PS[1][:, 0:32], ROWS[64:96, :], identf[64:96, 64:96])
        cp('act', POSC, PS[1][:, 0:32].rearrange("p (a b) -> p a b", a=2))
        for kv in range(2):
            for jc in range(2):
                for i in range(16):
                    mm(PS[1][:, 64 + kv * 2 + jc:65 + kv * 2 + jc], W1[:, kv, i, jc * 128:(jc + 1) * 128], POSC[:, kv, i:i + 1], i == 0, i == 15)
        tt('dve', PB1, PS[1][:, 64:68], COLA[:, 40:44], ALU.add)
        KC2 = ar.alloc([4, S], BF16)
        memset('pool', KC2[64:128, :, S - 1:S], 0.0)
        for j in range(4):
            for tc in range(4):
                bank = PS[2 + (j * 4 + tc) % 2]
                for kc in range(8):
                    mm(bank[:, :], wb[:, kc, j * 128:(j + 1) * 128], XNT[:, kc, tc * 512:(tc + 1) * 512], kc == 0, kc == 7)
                evac(KC2[0:64, j, tc * 512:(tc + 1) * 512], bank[0:64, :])
                if tc == 0:
                    evac(KC2[64:128, j, 0:511], bank[64:128, 1:512])
                else:
                    evac(KC2[64:128, j, tc * 512 - 1:tc * 512 + 511], bank[64:128, :])
        H1T = ar.alloc([2, 128], BF16)
        for kv in range(2):
            for g in range(2):
                j = kv * 2 + g
                for jc in range(2):
                    bank = PS[4 + jc]
                    for i in range(16):
                        rhs = KC2[:, j, 2 * i:2 * i + 16 * 126 + 1:16]
                        mm(bank[:, 0:127], W1[:, kv, i, jc * 128:(jc + 1) * 128], rhs, i == 0, i == 15)
                    act(H1T[:, jc, 0:127], bank[:, 0:127], AF.Gelu_apprx_tanh, bias=PB1[:, kv * 2 + jc:kv * 2 + jc + 1])
                if kv == 0:
                    bank = PS[6]
                    for jc in range(2):
                        mm(bank[:, 0:127], W2K[:, jc, :], H1T[:, jc, 0:127], jc == 0, jc == 1)
                    act(KCT[:, g, 0:127], bank[:, 0:127], AF.Identity, bias=COLA[:, 44:45])
                else:
                    bank = PS[7]
                    for jc in range(2):
                        mm(bank[0:127, 0:64], H1T[:, jc, 0:127], W2V[:, jc, :], jc == 0, False)
                    mm(bank[0:127, 0:64], onesb[0:1, 0:127], B2V[0:1, :], False, True)
                    cp('dve', VCA[0:127, g, 0:64], bank[0:127, 0:64])
        if stop == 'cmp':
            dump('kct', KCT, BF16)
            dump('vca', VCA, BF16)
            break
        ar.top = cmp_top
        QT = ar.alloc([4, S], BF16)
        KS = ar.alloc([2, S], BF16)
        KW = ar.alloc([2, S], BF16)
        VSf = ar.alloc([NT * 2 * 72], BF16)
        VWf = ar.alloc([NT * 2 * 72], BF16)
        memset('dve', VSf, 1.0)
        memset('dve', VWf, 1.0)
        VS = VSf.rearrange("p (t g d) -> p t g d", t=NT, g=2)
        VW = VWf.rearrange("p (t g d) -> p t g d", t=NT, g=2)
        proj_top = ar.top
        WB = [ar.alloc([8, 512], BF16) for _ in range(2)]
        wb = WB[1]
        fw.dma('pool', wb, win_v[:, :, 0:512])
        for pi in range(4):
            for tc in range(4):
                bank = PS[(pi * 4 + tc) % 2]
                for kc in range(8):
                    mm(bank[:, :], wb[:, kc, pi * 128:(pi + 1) * 128], XNT[:, kc, tc * 512:(tc + 1) * 512], kc == 0, kc == 7)
                evac(QT[:, pi, tc * 512:(tc + 1) * 512], bank[:, :])
        if stop == 'proj2':
            dump('qt', QT, BF16)
            break
        wb = WB[0]
        for j, c0 in enumerate([768, 832, 1024, 1088]):
            fw.dma('pool', wb[:, :, j * 128:j * 128 + 64], win_v[:, :, c0:c0 + 64])
            fw.dma('pool', wb[:, :, j * 128 + 64:j * 128 + 128], win_v[:, :, c0:c0 + 64])
        for j in range(4):
            dst = KS if j < 2 else KW
            for tc in range(4):
                bank = PS[2 + (j * 4 + tc) % 2]
                for kc in range(8):
                    mm(bank[:, :], wb[:, kc, j * 128:(j + 1) * 128], XNT[:, kc, tc * 512:(tc + 1) * 512], kc == 0, kc == 7)
                evac(dst[:, j % 2, tc * 512:(tc + 1) * 512], bank[:, :])
        if stop == 'proj3':
            dump('qt', QT, BF16)
            dump('ks', KS, BF16)
            break
        wb = WB[1]
        WT = ar.alloc([8, 320], BF16)
        fw.dma('pool', WT[:, :, 0:128], win_v[:, :, 896:1024])
        fw.dma('pool', WT[:, :, 128:256], win_v[:, :, 1152:1280])
        fw.dma('pool', WT[:, :, 256:320], win_v[:, :, 1280:1344])
        ZG = ar.alloc([24], F32)
        import os as _os
        for t in range(int(_os.environ.get('SUB4_NT', NT))):
            bank = PS[4 + t % 2]
            _m = int(_os.environ.get('SUB4_MODE', 9))
            if _m < 1:
                continue
            bks = [PS[2 + (t % 2) * 3 + i_] for i_ in range(3)]
            for gi_, (c0, c1) in enumerate([(0, 128), (128, 256), (256, 320)]):
                for kc in range(8):
                    mm(bks[gi_][:, 0:c1 - c0], XNT[:, kc, t * 128:(t + 1) * 128], WT[:, kc, c0:c1], kc == 0, kc == 7)
            if _m < 2:
                continue
            _ce = _os.environ.get('SUB4_ENG', 'dve')
            if _os.environ.get('SUB4_DST'):
                _tmpd = ar.alloc([64], BF16)
                for g in range(2):
                    cp(_ce, _tmpd, bank[:, g * 64:g * 64 + 64])
                    cp(_ce, _tmpd, bank[:, 128 + g * 64:128 + g * 64 + 64])
            else:
              for g in range(2):
                cp(_ce, VS[:, t, g, 0:64], bks[0][:, g * 64:g * 64 + 64])
                cp(_ce, VW[:, t, g, 0:64], bks[1][:, g * 64:g * 64 + 64])
            if _m < 3:
                continue
            tt('dve', ZG, bks[2][:, 0:24], GBB, ALU.add)
            if _m < 4:
                continue
            act(GATES[:, t, :], ZG, AF.Sigmoid)
        if stop == 'proj':
            dump('qt', QT, BF16)
            dump('gates', GATES)
            break
        ar.top = proj_top

        ar.top = xnt_off
        PT = [ar.alloc([512], BF16) for _ in range(3)]
        ON = ar.alloc([4, 512], F32)
        ONB = ar.alloc([4, 512], BF16)
        ONT = ar.alloc([4, S], BF16)
        assert ar.top <= xnt_off + 8 * S * 2
        ar.top = proj_top
        IMP = ar.alloc([4, 32], F32)
        TMPI = ar.alloc([4, 32], F32)
        TMPO = ar.alloc([4, 64], F32)
        DEN = ar.alloc([4], F32)
        CF = ar.alloc([4], F32)
        TOP8 = ar.alloc([8], F32)
        SEL = ar.alloc([4, 32], F32)
        NEGB = ar.alloc([4, 32], BF16)
        NEGBT = ar.alloc([512], BF16)
        junk2 = ar.alloc([512], BF16)
        memset('pool', NEGBT, 0.0)
        st_ctr = [0]
        acc_ctr = [0]

        def finish_branch(acc, qc, h, b, first):
            ts('dve', DEN, acc[:, :, 64], 1e-30, ALU.max)
            fw.op('dve', lambda e: e.reciprocal(out=DEN, in_=DEN), reads=[DEN], writes=[DEN])
            tt('dve', CF, DEN, GATES[:, 4 * qc:4 * qc + 4, 3 * h + b], ALU.mult)
            onv = ON[:, :, h * 64:(h + 1) * 64]
            if first:
                tt('dve', onv, acc[:, :, 0:64], CF.unsqueeze(2).to_broadcast([128, 4, 64]), ALU.mult)
            else:
                tt('dve', TMPO, acc[:, :, 0:64], CF.unsqueeze(2).to_broadcast([128, 4, 64]), ALU.mult)
                tt('pool', onv, onv, TMPO, ALU.add)

        for qc in range(4):
            for g in range(2):
                for r in range(4):
                    h = 4 * g + r
                    pi, half = h // 2, h % 2
                    p0 = half * 64
                    st = PS[st_ctr[0] % 3]
                    pt = PT[st_ctr[0] % 3]
                    st_ctr[0] += 1
                    mm(st[0:127, :], KCT[p0:p0 + 64, g, 0:127], QT[p0:p0 + 64, pi, qc * 512:(qc + 1) * 512], True, False)
                    mm(st[0:127, :], identb[0:127, 0:127], cmpb[0:127, qc * 512:(qc + 1) * 512], False, True)
                    act(pt[0:127, :], st[0:127, :], AF.Exp, scale=0.125)
                    accb = PS[3 + acc_ctr[0] % 2]
                    acc_ctr[0] += 1
                    acc = accb[:, 0:388].rearrange("p (i w) -> p i w", i=4)
                    for i in range(4):
                        mm(acc[:, i, :], pt[0:127, i * 128:(i + 1) * 128], VCA[0:127, g, :], i == 0, i == 3)
                    finish_branch(acc, qc, h, 0, True)
                    if r == 0:
                        tt('dve', IMP, acc[:, :, 65:97], DEN.unsqueeze(2).to_broadcast([128, 4, 32]), ALU.mult)
                    else:
                        tt('dve', TMPI, acc[:, :, 65:97], DEN.unsqueeze(2).to_broadcast([128, 4, 32]), ALU.mult)
                        tt('pool', IMP, IMP, TMPI, ALU.add)
                tt('dve', IMP, IMP, cbias[:, 4 * qc:4 * qc + 4, :], ALU.add)
                for i in range(4):
                    fw.op('dve', lambda e, i=i: e.max(out=TOP8, in_=IMP[:, i, :]), reads=[IMP[:, i, :]], writes=[TOP8])
                    ts('dve', SEL[:, i, :], IMP[:, i, :], TOP8[:, 7:8], ALU.is_ge)
                ts('dve', SEL, SEL, -1.0, ALU.add, -NEG, ALU.mult)
                tt('dve', NEGB, SEL, validb[:, 4 * qc:4 * qc + 4, :], ALU.add)
                pbt = PSB[5][:, 0:512]
                for i in range(4):
                    tr(pbt[0:32, i * 128:(i + 1) * 128], NEGB[:, i, :], identb)
                cp('act', NEGBT[0:32, :], pbt[0:32, :])
                if dbg and l == 0 and qc == 3 and g == 0:
                    dump('imp', IMP)
                    dump('negb', NEGB, BF16)
                items = []
                for r in range(4):
                    h = 4 * g + r
                    pi, half = h // 2, h % 2
                    p0 = half * 64
                    for b, KK, VV in ((1, KS, VS), (2, KW, VW)):
                        kb_lo = 0 if b == 1 else max(0, 4 * qc - 4)
                        kb_hi = 4 * qc + 3
                        accb = PS[3 + acc_ctr[0] % 2]
                        acc_ctr[0] += 1
                        acc = accb[:, 0:260].rearrange("p (i w) -> p i w", i=4)
                        for kb in range(kb_lo, kb_hi + 1):
                            qlo = max(kb, 4 * qc)
                            qhi = 4 * qc + 3 if b == 1 else min(kb + 4, 4 * qc + 3)
                            c0 = (qlo - 4 * qc) * 128
                            c1 = (qhi - 4 * qc + 1) * 128
                            bi = st_ctr[0] % 3
                            st_ctr[0] += 1
                            items.append(dict(st=PS[bi], pt=PT[bi], acc=acc, kb=kb, qlo=qlo, qhi=qhi, c0=c0, c1=c1, b=b,
                                              KK=KK, VV=VV, p0=p0, pi=pi, h=h, first=(kb == kb_lo), last=(kb == kb_hi)))

                def emit_S(it):
                    st, c0, c1, kb, p0, pi, b = it['st'], it['c0'], it['c1'], it['kb'], it['p0'], it['pi'], it['b']
                    mm(st[:, c0:c1], it['KK'][p0:p0 + 64, g, kb * 128:(kb + 1) * 128], QT[p0:p0 + 64, pi, qc * 512 + c0:qc * 512 + c1], True, False)
                    if b == 1:
                        mm(st[:, c0:c1], emat[0:32, kb * 128:(kb + 1) * 128], NEGBT[0:32, c0:c1], False, False)
                    if kb >= 4 * qc:
                        mm(st[:, c0:c0 + 128], identb, tri1, False, False)
                    if b == 2 and kb + 4 <= 4 * qc + 3:
                        mm(st[:, c1 - 128:c1], identb, tri2, False, False)

                def emit_E(it):
                    act(it['pt'][:, it['c0']:it['c1']], it['st'][:, it['c0']:it['c1']], AF.Exp, scale=0.125)

                def emit_P(it):
                    for qb in range(it['qlo'], it['qhi'] + 1):
                        i = qb - 4 * qc
                        mm(it['acc'][:, i, :], it['pt'][:, i * 128:(i + 1) * 128], it['VV'][:, it['kb'], g, 0:65],
                           it['first'] and qb == it['qlo'], False)
                    if it['last']:
                        finish_branch(it['acc'], qc, it['h'], it['b'], False)

                n_it = len(items)
                for k in range(min(2, n_it)):
                    emit_S(items[k])
                for k in range(n_it):
                    emit_E(items[k])
                    emit_P(items[k])
                    if k + 2 < n_it:
                        emit_S(items[k + 2])
            for i in range(4):
                act(junk2, ON[:, i, :], AF.Square, accum=SSN[:, 4 * qc + i:4 * qc + i + 1])
            cp('pool', ONB, ON)
            for cc in range(4):
                pb = PSB[6 + cc % 2][:, 0:512]
                for i in range(4):
                    tr(pb[:, i * 128:(i + 1) * 128], ONB[:, i, cc * 128:(cc + 1) * 128], identb)
                act(ONT[:, cc, qc * 512:(qc + 1) * 512], pb, AF.Identity, scale=COLA[:, 36 + cc:37 + cc])
        rstd_from_ss(SSN, RSN, 512)
        if stop == 'attn':
            dump('ont', ONT, BF16)
            dump('rsn', RSN)
            dump('kct', KCT, BF16)
            dump('vca', VCA, BF16)
            dump('qt', QT, BF16)
            dump('gates', GATES)
            break

        ar.top = cmp_top
        OLT2 = ar.alloc([4, S], BF16)
        fw.dma('sp', OLT2, olt_d)
        WO = ar.alloc([8, D], BF16)
        fw.dma('pool', WO[:, 0:4, :], W['w_out'][l].rearrange("(kc p) n -> p kc n", p=128)[:, 0:4, :])
        fw.dma('pool', WO[:, 4:8, :], W['w_out'][l].rearrange("(kc p) n -> p kc n", p=128)[:, 4:8, :])
        for kc in range(8):
            tt('pool', WO[:, kc, :], WO[:, kc, :], MOD[:, 2, :], ALU.mult)
        for t in range(NT):
            for nh in range(2):
                b1 = PS[(t * 2 + nh) % 2]
                b2 = PS[2 + (t * 2 + nh) % 2]
                for cc in range(4):
                    mm(b1[:, :], ONT[:, cc, t * 128:(t + 1) * 128], WO[:, cc, nh * 512:(nh + 1) * 512], cc == 0, cc == 3)
                for cc in range(4):
                    mm(b2[:, :], OLT2[:, cc, t * 128:(t + 1) * 128], WO[:, 4 + cc, nh * 512:(nh + 1) * 512], cc == 0, cc == 3)
                xv = X[:, t, nh * 512:(nh + 1) * 512]
                stt(xv, b1[:, :], RSN[:, t:t + 1], xv, ALU.mult, ALU.add)
                stt(xv, b2[:, :], RSL[:, t:t + 1], xv, ALU.mult, ALU.add)
        if stop == 'mixer':
            dump('x1', X)
            break

        ar.top = mix_top
        MOD = ar.alloc([3, D], F32)
        XNT = ar.alloc([8, S], BF16)
        mark_ = ar.top
        GN = ar.alloc([D], F32)
        fw.dma('sp', GN, W['ffn_norm_g'][l].partition_broadcast(128))
        mod_half(l, 1, MOD, GN)
        ar.top = mark_
        norm_to_xnt(XNT, MOD[:, 1, :], MOD[:, 0, :])
        fw.dma('sp', ROWS[0:66, :], W['ffn_conv_w'][l].rearrange("k (m p) -> (k m) p", p=128))
        fw.dma('sp', ROWS[66:88, :], W['ffn_conv_b'][l].rearrange("(m p) -> m p", p=128))
        tr(PS[0][:, 0:88], ROWS[0:88, :], identf[0:88, 0:88])
        cp('act', COLB[:, 0:88], PS[0][:, 0:88])
        HMT = ar.alloc([5, S], BF16)
        WD = [ar.alloc([5, D], BF16) for _ in range(2)]
        WGU = [ar.alloc([2, 8, 256], BF16) for _ in range(2)]
        GRAW = ar.alloc([2 + S], F32)
        CT = [ar.alloc([512], F32) for _ in range(2)]
        SG = [ar.alloc([512], F32) for _ in range(2)]
        memset('dve', GRAW[:, 0:2], 0.0)
        wg_v = W['ffn_w_gate'][l].rearrange("(kc p) n -> p kc n", p=128)
        wu_v = W['ffn_w_up'][l].rearrange("(kc p) n -> p kc n", p=128)
        wd_v = W['ffn_w_down'][l].rearrange("(m p) n -> p m n", p=128)
        slab_ctr = [0]

        def load_slab(m0, n):
            sl = WGU[slab_ctr[0] % 2]
            slab_ctr[0] += 1
            fw.dma('pool', sl[:, 0, :, 0:n * 128], wg_v[:, :, m0 * 128:(m0 + n) * 128])
            fw.dma('pool', sl[:, 1, :, 0:n * 128], wu_v[:, :, m0 * 128:(m0 + n) * 128])
            return sl

        slabs = [(m0, min(2, NM - m0)) for m0 in range(0, NM, 2)]
        slab_bufs = {}
        slab_bufs[0] = load_slab(*slabs[0])
        ck = 0
        for gi, (gm0, gn) in enumerate(FFN_GROUPS):
            wd = WD[gi % 2]
            fw.dma('pool', wd[:, 0:gn, :], wd_v[:, gm0:gm0 + gn, :])
            for mloc in range(gn):
                tt('pool', wd[:, mloc, :], wd[:, mloc, :], MOD[:, 2, :], ALU.mult)
            for mloc in range(gn):
                m = gm0 + mloc
                si = m // 2
                if m % 2 == 0 and si + 1 < len(slabs):
                    slab_bufs[si + 1] = load_slab(*slabs[si + 1])
                sl = slab_bufs[si]
                sc = (m % 2) * 128
                w0 = COLB[:, 0 * NM + m:0 * NM + m + 1]
                w1 = COLB[:, 1 * NM + m:1 * NM + m + 1]
                w2 = COLB[:, 2 * NM + m:2 * NM + m + 1]
                cb = COLB[:, 66 + m:67 + m]
                for tc in range(4):
                    bg = PS[(ck % 2) * 2]
                    bu = PS[(ck % 2) * 2 + 1]
                    ct = CT[ck % 2]
                    sg = SG[ck % 2]
                    ck += 1
                    for kc in range(8):
                        mm(bg[:, :], sl[:, 0, kc, sc:sc + 128], XNT[:, kc, tc * 512:(tc + 1) * 512], kc == 0, kc == 7)
                    for kc in range(8):
                        mm(bu[:, :], sl[:, 1, kc, sc:sc + 128], XNT[:, kc, tc * 512:(tc + 1) * 512], kc == 0, kc == 7)
                    if tc == 0:
                        pass
                    cp('act', GRAW[:, 2 + tc * 512:2 + (tc + 1) * 512], bg[:, :])
                    act(ct, bg[:, :], AF.Identity, bias=cb, scale=w2)
                    stt(ct, GRAW[:, tc * 512 + 1:tc * 512 + 513], w1, ct, ALU.mult, ALU.add)
                    stt(ct, GRAW[:, tc * 512:tc * 512 + 512], w0, ct, ALU.mult, ALU.add)
                    act(sg, ct, AF.Silu)
                    tt('dve', HMT[:, mloc, tc * 512:(tc + 1) * 512], sg, bu[:, :], ALU.mult)
            for t in range(NT):
                for nh in range(2):
                    bd = PS[4 + (t * 2 + nh) % 3]
                    for mloc in range(gn):
                        mm(bd[:, :], HMT[:, mloc, t * 128:(t + 1) * 128], wd[:, mloc, nh * 512:(nh + 1) * 512], mloc == 0, mloc == gn - 1)
                    xv = X[:, t, nh * 512:(nh + 1) * 512]
                    tt('dve', xv, bd[:, :], xv, ALU.add)
        if stop == 'ffn':
            dump('x2', X)
            break

    if stop is None:
        ar.top = setup_top
        GF = ar.alloc([D], F32)
        fw.dma('sp', GF, W['final_norm_g'].partition_broadcast(128))
        junk = ar.alloc([D], BF16)
        OB = [ar.alloc([D], F32) for _ in range(2)]
        for t in range(NT):
            act(junk, X[:, t, :], AF.Square, accum=SS[:, t:t + 1])
        rstd_from_ss(SS, RS, D)
        for t in range(NT):
            ob = OB[t % 2]
            stt(ob, X[:, t, :], RS[:, t:t + 1], GF, ALU.mult, ALU.mult)
            fw.dma('sp', out_d[t * 128:(t + 1) * 128, :], ob, is_output=True)
    fw.finish('sp')
    fw.emit()
    nc._dbg_outs = dbg_outs
    nc._fw = fw
    nc._arena_peak = ar.peak
    return nc


_CACHE = {}


def make_in_maps(inputs, n_layers=L):
    consts = make_consts()
    x = np.ascontiguousarray(np.asarray(inputs['x'], dtype=np.float32))
    c = np.ascontiguousarray(np.asarray(inputs['c'], dtype=np.float32))
    shared = {}
    for nm, shp in WEIGHT_SPECS:
        a = np.asarray(inputs[nm], dtype=np.float32)
        if nm != 'final_norm_g':
            a = a[0:n_layers]
        shared[nm] = np.ascontiguousarray(a)
    shared.update(consts)
    in_maps = []
    for b in range(8):
        m = dict(shared)
        m['x'] = x[b]
        m['c'] = c[b].reshape(8, 128)
        in_maps.append(m)
    return in_maps


def kernel(**inputs):
    if 'nc' not in _CACHE:
        _CACHE['nc'] = build_program()
    nc = _CACHE['nc']
    in_maps = make_in_maps(inputs)
    res = run_bass_kernel_spmd(nc, in_maps, core_ids=list(range(8)))
    out = np.stack([np.asarray(r['out'], dtype=np.float32) for r in res.results], axis=0)
    return out
```

```python
import numpy as np
import ml_dtypes
import concourse.bass as bass
import concourse.mybir as mybir
from concourse.bass_utils import run_bass_kernel_spmd

F32 = mybir.dt.float32
BF16 = mybir.dt.bfloat16
AF = mybir.ActivationFunctionType
ALU = mybir.AluOpType

L = 4
D = 1024
S = 2048
NT = 16
DFF = 2816
NM = 22
INC = 2328
NEG = -30000.0
FFN_GROUPS = [(0, 5), (5, 5), (10, 4), (14, 4), (18, 4)]


class _IMap:
    def __init__(self):
        self.recs = []

    def access(self, lo, hi, tok, is_write, deps):
        out = []
        for rec in self.recs:
            rlo, rhi, w, rd = rec
            if rhi <= lo or rlo >= hi:
                out.append(rec)
                continue
            if w is not None:
                deps.append(w)
            if is_write:
                deps.extend(rd.values())
                if rlo < lo:
                    out.append([rlo, lo, w, dict(rd)])
                if rhi > hi:
                    out.append([hi, rhi, w, dict(rd)])
            else:
                if rlo < lo:
                    out.append([rlo, lo, w, dict(rd)])
                if rhi > hi:
                    out.append([hi, rhi, w, dict(rd)])
                nrd = dict(rd)
                k = id(tok[0])
                if k not in nrd or nrd[k][1] < tok[1]:
                    nrd[k] = tok
                out.append([max(lo, rlo), min(hi, rhi), w, nrd])
        if is_write:
            out.append([lo, hi, tok, {}])
        else:
            covered = sorted((r[0], r[1]) for r in out if not (r[1] <= lo or r[0] >= hi))
            cur = lo
            for a, b in covered:
                if a > cur:
                    out.append([cur, a, None, {id(tok[0]): tok}])
                cur = max(cur, b)
            if cur < hi:
                out.append([cur, hi, None, {id(tok[0]): tok}])
        self.recs = out


def _esize(dt):
    if dt == F32:
        return 4
    if dt == BF16:
        return 2
    return mybir.dt.size(dt)


def ap_intervals(ap):
    t = ap.tensor
    es = _esize(ap.dtype)
    dims = [list(d) for d in ap.ap]
    space = str(ap.space).upper()
    if 'DRAM' in space or 'HBM' in space:
        off = ap.offset
        fd = dims
        key = 'D:' + t.name
    else:
        pstride = dims[0][0]
        off = ap.offset % pstride if pstride > 0 else ap.offset
        fd = dims[1:]
        key = ('P:' if 'PSUM' in space else 'S:') + t.name
    fd = [d for d in fd if d[1] > 1 and d[0] != 0]
    fd.sort(key=lambda d: -abs(d[0]))
    ivs = [(off, off + 1)]
    for step, cnt in reversed(fd):
        step = abs(step)
        ext = ivs[-1][1] - ivs[0][0]
        if step <= ext or len(ivs) * cnt > 40:
            ivs = [(ivs[0][0], ivs[-1][1] + (cnt - 1) * step)]
        else:
            n = []
            for i in range(cnt):
                for (a, b) in ivs:
                    n.append((a + i * step, b + i * step))
            ivs = n
    return key, [(a * es, b * es) for a, b in ivs]


class FW:
    ENGS = ['pe', 'act', 'dve', 'pool', 'sp']
    EPOCH = 30000

    def __init__(self, nc, n_dma_sems=12):
        self.nc = nc
        self.prog = {e: [] for e in self.ENGS}
        self.sem = {e: nc.alloc_semaphore(name='c_' + e) for e in self.ENGS}
        self.cnt = {e: 0 for e in self.ENGS}
        self.tot = {e: 0 for e in self.ENGS}
        self.seen = {e: {} for e in self.ENGS}
        self.maps = {}
        self.dsem = {}
        for q in ['sp', 'pool']:
            self.dsem[q] = [[nc.alloc_semaphore(name=f'd_{q}{i}'), 0] for i in range(n_dma_sems)]
        self.drr = {q: 0 for q in self.dsem}
        self.out_toks = []
        self.last_tok = {}

    def _deps(self, tok, reads, writes):
        deps = []
        for ap in reads:
            key, ivs = ap_intervals(ap)
            m = self.maps.setdefault(key, _IMap())
            for lo, hi in ivs:
                m.access(lo, hi, tok, False, deps)
        for ap in writes:
            key, ivs = ap_intervals(ap)
            m = self.maps.setdefault(key, _IMap())
            for lo, hi in ivs:
                m.access(lo, hi, tok, True, deps)
        return deps

    def _waits(self, e, deps, tok=None, skip_self=False):
        need = {}
        for d in deps:
            if tok is not None and d[0] is tok[0] and d[1] >= tok[1]:
                continue
            if skip_self and tok is not None and d[0] is tok[0]:
                continue
            k = id(d[0])
            if k not in need or need[k][1] < d[1]:
                need[k] = d
        w = []
        for k, d in need.items():
            if self.seen[e].get(k, 0) >= d[1]:
                continue
            self.seen[e][k] = d[1]
            w.append((d[0], d[1]))
        return w

    def op(self, e, fn, reads=(), writes=()):
        if self.cnt[e] >= self.EPOCH:
            self.last_tok[e] = (self.sem[e], self.cnt[e])
            self.sem[e] = self.nc.alloc_semaphore(name=f'c_{e}_{self.tot[e]}')
            self.cnt[e] = 0
        self.cnt[e] += 1
        self.tot[e] += 1
        tok = (self.sem[e], self.cnt[e])
        deps = self._deps(tok, reads, writes)
        w = self._waits(e, deps, tok, skip_self=(e == 'pe'))
        self.prog[e].append((w, fn, (self.sem[e], 1)))
        return tok

    def dma(self, q, out, in_, is_output=False, **kw):
        slot = self.dsem[q][self.drr[q] % len(self.dsem[q])]
        self.drr[q] += 1
        sem = slot[0]
        prev = slot[1]
        slot[1] += 16
        tok = (sem, slot[1])
        deps = self._deps(tok, [in_], [out])
        if prev > 0:
            deps.append((sem, prev))
        w = self._waits(q, deps, tok)
        self.prog[q].append((w, lambda eng: eng.dma_start(out=out, in_=in_, **kw), (sem, 16)))
        if is_output:
            self.out_toks.append(tok)
        return tok

    def finish(self, e='sp'):
        deps = list(self.out_toks)
        for q in self.dsem:
            for sem, v in self.dsem[q]:
                if v > 0:
                    deps.append((sem, v))
        for x in self.ENGS:
            if x != e and self.cnt[x] > 0:
                deps.append((self.sem[x], self.cnt[x]))
        w = self._waits(e, deps)
        self.prog[e].append((w, None, None))

    def emit(self):
        nc = self.nc
        with nc.Block() as block:
            def mk(e):
                def body(eng):
                    for w, fn, inc in self.prog[e]:
                        for sem, val in w:
                            eng.wait_ge(sem, val)
                        if fn is not None:
                            ins = fn(eng)
                            ins.then_inc(inc[0], inc[1])
                return body
            block.tensor(mk('pe'))
            block.scalar(mk('act'))
            block.vector(mk('dve'))
            block.gpsimd(mk('pool'))
            block.sync(mk('sp'))


class Arena:
    def __init__(self, nc, nbytes):
        self.t = nc.alloc_sbuf_tensor("arena", [128, nbytes // 2], BF16)
        self.top = 0
        self.cap = nbytes
        self.peak = 0

    def alloc(self, free_shape, dtype):
        n = 1
        for s in free_shape:
            n *= s
        nb = n * _esize(dtype)
        nb_al = (nb + 63) // 64 * 64
        off = self.top
        self.top += nb_al
        self.peak = max(self.peak, self.top)
        assert self.top <= self.cap, f"arena overflow {self.top} > {self.cap}"
        a = self.t[:, off // 2: off // 2 + nb // 2]
        if dtype != BF16:
            a = a.bitcast(dtype)
        if len(free_shape) == 2:
            a = a.rearrange("p (a b) -> p a b", a=free_shape[0])
        elif len(free_shape) == 3:
            a = a.rearrange("p (a b c) -> p a b c", a=free_shape[0], b=free_shape[1])
        return a


def make_consts():
    bf = ml_dtypes.bfloat16
    c = {}
    c['identb'] = np.eye(128, dtype=np.float32).astype(bf)
    c['identf'] = np.eye(128, dtype=np.float32)
    kk = np.arange(128)[:, None]
    tt = np.arange(128)[None, :]
    c['tri1'] = np.where(kk <= tt, 0.0, NEG).astype(bf)
    c['tri2'] = np.where(kk > tt, 0.0, NEG).astype(bf)
    cc = np.arange(128)[:, None]
    t = np.arange(S)[None, :]
    c['cmpb'] = np.where((16 * cc + 31 <= t) & (cc < 127), 0.0, NEG).astype(bf)
    e64 = np.zeros((128, S), np.float32)
    E = (np.arange(S)[None, :] // 64 == np.arange(32)[:, None]).astype(np.float32)
    e64[0:32] = E
    e64[64:96] = E
    c['emat'] = e64.astype(bf)
    t = np.arange(S)
    qblk = t // 64
    j = np.arange(32)
    valid = j[None, :] <= qblk[:, None]
    forced = (j[None, :] == 0) | (j[None, :] == qblk[:, None]) | (j[None, :] == qblk[:, None] - 1)
    cbias = np.where(valid, np.where(forced, 1e4, 0.0), -1e30).astype(np.float32)
    validb = np.where(valid, 0.0, NEG).astype(np.float32)
    c['cbias'] = np.ascontiguousarray(cbias.reshape(NT, 128, 32).transpose(1, 0, 2))
    c['validb'] = np.ascontiguousarray(validb.reshape(NT, 128, 32).transpose(1, 0, 2))
    cs = np.arange(128) * 16
    js = np.arange(32) * 64
    ov = ((cs[:, None] < js[None, :] + 64) & (cs[:, None] + 32 > js[None, :])).astype(np.float32)
    ov[127] = 0
    vca = np.zeros((128, 33), np.float32)
    vca[:, 0] = 1.0
    vca[:, 1:] = ov
    c['vcaux'] = vca.astype(bf)
    return c


WEIGHT_SPECS = [
    ('ada_w', [L, D, 6 * D]), ('ada_b', [L, 6 * D]), ('mix_norm_g', [L, D]), ('ffn_norm_g', [L, D]),
    ('w_in', [L, D, INC]), ('nsa_gate_b', [L, 24]), ('cmp_pos', [L, 2, 32, 64]), ('cmp_w1', [L, 2, 2048, 256]),
    ('cmp_b1', [L, 2, 256]), ('cmp_w2', [L, 2, 256, 64]), ('cmp_b2', [L, 2, 64]), ('lru_conv_w', [L, 4, 512]),
    ('lru_conv_b', [L, 512]), ('lru_wa', [L, 8, 64, 64]), ('lru_ba', [L, 512]), ('lru_wx', [L, 8, 64, 64]),
    ('lru_bx', [L, 512]), ('lru_lambda', [L, 512]), ('nsa_out_norm_g', [L, 512]), ('lru_out_norm_g', [L, 512]),
    ('w_out', [L, D, D]), ('ffn_w_gate', [L, D, DFF]), ('ffn_w_up', [L, D, DFF]), ('ffn_conv_w', [L, 3, DFF]),
    ('ffn_conv_b', [L, DFF]), ('ffn_w_down', [L, DFF, D]), ('final_norm_g', [D]),
]
CONST_SPECS = [('identb', [128, 128], BF16), ('identf', [128, 128], F32), ('tri1', [128, 128], BF16),
               ('tri2', [128, 128], BF16), ('cmpb', [128, S], BF16), ('emat', [128, S], BF16),
               ('cbias', [128, NT, 32], F32), ('validb', [128, NT, 32], F32), ('vcaux', [128, 33], BF16)]


def build_program(n_layers=L, stop=None, dbg=False):
    nc = bass.Bass("TRN2", target_bir_lowering=False)
    fw = FW(nc)
    W = {}
    x_d = nc.dram_tensor("x", [S, D], F32, kind="ExternalInput").ap()
    c_d = nc.dram_tensor("c", [8, 128], F32, kind="ExternalInput").ap()
    for nm, shp in WEIGHT_SPECS:
        shp = list(shp)
        if len(shp) > 1 or nm != 'final_norm_g':
            shp[0] = n_layers
        W[nm] = nc.dram_tensor(nm, shp, F32, kind="ExternalInput").ap()
    CD = {}
    for nm, shp, dt in CONST_SPECS:
        CD[nm] = nc.dram_tensor(nm, shp, dt, kind="ExternalInput").ap()
    out_d = nc.dram_tensor("out", [S, D], F32, kind="ExternalOutput").ap()
    olt_d = nc.dram_tensor("olt_scr", [128, 4, S], BF16, kind="Internal").ap()
    dbg_outs = {}

    ar = Arena(nc, 207 * 1024)
    PS = [nc.alloc_psum_tensor(f"bank{i}", [128, 512], F32) for i in range(8)]
    PSB = [p[:, :].bitcast(BF16) for p in PS]

    def mm(out, lhsT, rhs, start, stop):
        fw.op('pe', lambda e: e.matmul(out, lhsT=lhsT, rhs=rhs, start=start, stop=stop, skip_group_check=True),
              reads=[lhsT, rhs], writes=[out])

    def tr(out, in_, ident):
        fw.op('pe', lambda e: e.transpose(out=out, in_=in_, identity=ident), reads=[in_, ident], writes=[out])

    def act(out, in_, func, bias=None, scale=None, accum=None):
        kw = {}
        rd = [in_]
        wr = [out]
        if bias is not None:
            kw['bias'] = bias
            if not isinstance(bias, float):
                rd.append(bias)
        if scale is not None:
            kw['scale'] = scale
            if not isinstance(scale, float):
                rd.append(scale)
        if accum is not None:
            kw['accum_out'] = accum
            wr.append(accum)
        fw.op('act', lambda e: e.activation(out=out, in_=in_, func=func, **kw), reads=rd, writes=wr)

    def tt(eng, out, in0, in1, op):
        fw.op(eng, lambda e: e.tensor_tensor(out=out, in0=in0, in1=in1, op=op), reads=[in0, in1], writes=[out])

    def ts(eng, out, in0, s1, op0, s2=None, op1=None):
        rd = [in0]
        if not isinstance(s1, float):
            rd.append(s1)
        if s2 is not None and not isinstance(s2, float):
            rd.append(s2)
        if op1 is None:
            fw.op(eng, lambda e: e.tensor_scalar(out=out, in0=in0, scalar1=s1, scalar2=None, op0=op0), reads=rd, writes=[out])
        else:
            fw.op(eng, lambda e: e.tensor_scalar(out=out, in0=in0, scalar1=s1, scalar2=s2, op0=op0, op1=op1), reads=rd, writes=[out])

    def stt(out, in0, scalar, in1, op0, op1):
        rd = [in0, in1]
        if not isinstance(scalar, float):
            rd.append(scalar)
        fw.op('dve', lambda e: e.scalar_tensor_tensor(out=out, in0=in0, scalar=scalar, in1=in1, op0=op0, op1=op1), reads=rd, writes=[out])

    def cp(eng, out, in_):
        if eng == 'act':
            fw.op('act', lambda e: e.copy(out=out, in_=in_), reads=[in_], writes=[out])
        else:
            fw.op(eng, lambda e: e.tensor_copy(out=out, in_=in_), reads=[in_], writes=[out])

    def memset(eng, out, val):
        fw.op(eng, lambda e: e.memset(out, val), writes=[out])

    ev_ctr = [0]

    def evac(out, in_):
        ev_ctr[0] += 1
        cp('act' if ev_ctr[0] % 2 else 'dve', out, in_)

    def dump(name, ap, dt=F32):
        if not dbg:
            return
        shp = list(ap.shape)
        d = nc.dram_tensor("dbg_" + name, shp, dt, kind="ExternalOutput").ap()
        dbg_outs[name] = shp
        fw.dma('sp', d, ap, is_output=True)

    X = ar.alloc([NT, D], F32)
    identb = ar.alloc([128], BF16)
    identf = ar.alloc([128], F32)
    tri1 = ar.alloc([128], BF16)
    tri2 = ar.alloc([128], BF16)
    cmpb = ar.alloc([S], BF16)
    emat = ar.alloc([S], BF16)
    cbias = ar.alloc([NT, 32], F32)
    validb = ar.alloc([NT, 32], F32)
    vcaux = ar.alloc([33], BF16)
    onesb = ar.alloc([128], BF16)
    epsc = ar.alloc([1], F32)
    CREP = ar.alloc([8, 128], BF16)
    ROWS = ar.alloc([128], F32)
    COLA = ar.alloc([64], F32)
    COLB = ar.alloc([96], F32)
    CL = ar.alloc([4], F32)
    WABD = ar.alloc([4, 128], BF16)
    WXBD = ar.alloc([4, 128], BF16)
    SS = ar.alloc([NT], F32)
    RS = ar.alloc([NT], F32)
    SSL = ar.alloc([NT], F32)
    SSN = ar.alloc([NT], F32)
    RSL = ar.alloc([NT], F32)
    RSN = ar.alloc([NT], F32)
    GATES = ar.alloc([NT, 24], F32)
    GBB = ar.alloc([24], F32)
    HLAST = ar.alloc([1], F32)
    base_top = ar.top

    for i in range(4):
        fw.dma('sp', X[:, 4 * i:4 * i + 4, :], x_d.rearrange("(t p) d -> p t d", p=128)[:, 4 * i:4 * i + 4, :])
    for nm, ap_ in [('identb', identb), ('identf', identf), ('tri1', tri1), ('tri2', tri2), ('cmpb', cmpb),
                    ('emat', emat), ('cbias', cbias), ('validb', validb), ('vcaux', vcaux)]:
        fw.dma('sp', ap_, CD[nm])
    memset('dve', onesb, 1.0)
    memset('dve', epsc, 1e-6)
    memset('pool', WABD, 0.0)
    memset('pool', WXBD, 0.0)
    fw.dma('sp', ROWS[0:8, :], c_d)
    tr(PS[0][:, 0:8], ROWS[0:8, :], identf[0:8, 0:8])
    ccol = ar.alloc([8], F32)
    ccolb = ar.alloc([8], BF16)
    act(ccol, PS[0][:, 0:8], AF.Silu)
    cp('dve', ccolb, ccol)
    for kc in range(8):
        cp('dve', CREP[:, kc, :], ccolb[:, kc:kc + 1].to_broadcast([128, 128]))
    setup_top = ar.top

    def rstd_from_ss(ss_ap, out_ap, n):
        act(out_ap, ss_ap, AF.Sqrt, bias=epsc[:, 0:1], scale=1.0 / n)
        fw.op('dve', lambda e: e.reciprocal(out=out_ap, in_=out_ap), reads=[out_ap], writes=[out_ap])

    def mod_half(l, half, MOD, gnorm_b):
        mark = ar.top
        ADAW = [ar.alloc([8, 512], BF16) for _ in range(2)]
        ADAB = [ar.alloc([512], BF16) for _ in range(2)]
        for ch in range(6):
            col0 = (half * 6 + ch) * 512
            aw = ADAW[ch % 2]
            ab = ADAB[ch % 2]
            fw.dma('pool', aw, W['ada_w'][l].rearrange("(kc p) n -> p kc n", p=128)[:, :, col0:col0 + 512])
            fw.dma('pool', ab[0:1, :], W['ada_b'][l:l + 1, col0:col0 + 512])
            bank = PS[ch % 2]
            for kc in range(8):
                mm(bank[:, :], CREP[:, kc, :], aw[:, kc, :], kc == 0, False)
            mm(bank[:, :], onesb[0:1, :], ab[0:1, :], False, True)
            which = ch // 2
            dst = MOD[:, which, (ch % 2) * 512:(ch % 2) * 512 + 512]
            if which == 1:
                stt(dst, bank[:, :], 1.0, gnorm_b[:, (ch % 2) * 512:(ch % 2) * 512 + 512], ALU.add, ALU.mult)
            else:
                cp('act', dst, bank[:, :])
        ar.top = mark

    def norm_to_xnt(XNT, gs_b, sh_b):
        mark = ar.top
        junk = ar.alloc([D], BF16)
        TMP = [ar.alloc([D], F32) for _ in range(2)]
        HB = [ar.alloc([D], BF16) for _ in range(2)]
        for t in range(NT):
            act(junk, X[:, t, :], AF.Square, accum=SS[:, t:t + 1])
        rstd_from_ss(SS, RS, D)
        for t in range(NT):
            tmp = TMP[t % 2]
            hb = HB[t % 2]
            stt(tmp, X[:, t, :], RS[:, t:t + 1], gs_b, ALU.mult, ALU.mult)
            tt('pool', hb, tmp, sh_b, ALU.add)
            pb = PSB[6 + (t % 2)].rearrange("p (a b) -> p a b", a=8)
            for kc in range(8):
                tr(pb[:, kc, :], hb[:, kc * 128:(kc + 1) * 128], identb)
            evac(XNT[:, :, t * 128:(t + 1) * 128], pb)
        ar.top = mark

    for l in range(n_layers):
        ar.top = setup_top
        fw.dma('sp', ROWS[0:16, :], W['lru_conv_w'][l].rearrange("k (ci p) -> (k ci) p", p=128))
        for i, nm in enumerate(['lru_conv_b', 'lru_ba', 'lru_bx', 'lru_lambda', 'lru_out_norm_g', 'nsa_out_norm_g']):
            fw.dma('sp', ROWS[16 + 4 * i:20 + 4 * i, :], W[nm][l].rearrange("(ci p) -> ci p", p=128))
        fw.dma('sp', ROWS[40:44, :], W['cmp_b1'][l].rearrange("kv (jc p) -> (kv jc) p", p=128))
        fw.dma('sp', ROWS[44:45, 0:64], W['cmp_b2'][l, 0:1, :])
        fw.dma('sp', ROWS[44:45, 64:128], W['cmp_b2'][l, 0:1, :])
        tr(PS[0][:, 0:45], ROWS[0:45, :], identf[0:45, 0:45])
        cp('act', COLA[:, 0:45], PS[0][:, 0:45])
        act(CL, COLA[:, 28:32], AF.Exp, scale=-1.0)
        act(CL, CL, AF.Ln, bias=1.0)
        ts('dve', CL, CL, -8.0, ALU.mult)
        for n in range(8):
            ci, hh = n // 2, n % 2
            fw.dma('pool', WABD[hh * 64:hh * 64 + 64, ci, hh * 64:hh * 64 + 64], W['lru_wa'][l, n])
            fw.dma('pool', WXBD[hh * 64:hh * 64 + 64, ci, hh * 64:hh * 64 + 64], W['lru_wx'][l, n])
        fw.dma('sp', GBB, W['nsa_gate_b'][l].partition_broadcast(128))

        mix_top = ar.top
        MOD = ar.alloc([3, D], F32)
        xnt_off = ar.top
        XNT = ar.alloc([8, S], BF16)
        mark_ = ar.top
        GN = ar.alloc([D], F32)
        fw.dma('sp', GN, W['mix_norm_g'][l].partition_broadcast(128))
        mod_half(l, 0, MOD, GN)
        ar.top = mark_
        norm_to_xnt(XNT, MOD[:, 1, :], MOD[:, 0, :])
        if stop == 'norm1':
            dump('xnt', XNT, BF16)
            dump('mod', MOD)
            break

        lru_top = ar.top
        OLT = ar.alloc([4, S], BF16)
        WL = ar.alloc([8, 1024], BF16)
        fw.dma('pool', WL[:, :, 0:512], W['w_in'][l].rearrange("(kc p) n -> p kc n", p=128)[:, :, 1304:1816])
        fw.dma('pool', WL[:, :, 512:1024], W['w_in'][l].rearrange("(kc p) n -> p kc n", p=128)[:, :, 1816:2328])
        TL = 1024
        ZX = ar.alloc([3 + TL], F32)
        XC = ar.alloc([TL], F32)
        GY = ar.alloc([TL], F32)
        A_ = ar.alloc([TL], F32)
        I_ = ar.alloc([TL], F32)
        T_ = ar.alloc([TL], F32)
        XCB = ar.alloc([TL], BF16)
        OSQ = ar.alloc([TL], BF16)
        for ci in range(4):
            cw = [COLA[:, k * 4 + ci:k * 4 + ci + 1] for k in range(4)]
            cb = COLA[:, 16 + ci:17 + ci]
            ba = COLA[:, 20 + ci:21 + ci]
            bx = COLA[:, 24 + ci:25 + ci]
            gl = COLA[:, 32 + ci:33 + ci]
            for hf in range(2):
                if hf == 0:
                    memset('dve', ZX[:, 0:3], 0.0)
                else:
                    cp('dve', ZX[:, 0:3], ZX[:, TL:TL + 3])
                for tci in range(2):
                    tok0 = hf * TL + tci * 512
                    for kc in range(8):
                        mm(PS[tci][:, :], WL[:, kc, ci * 128:(ci + 1) * 128], XNT[:, kc, tok0:tok0 + 512], kc == 0, kc == 7)
                    for kc in range(8):
                        mm(PS[2 + tci][:, :], WL[:, kc, 512 + ci * 128:512 + (ci + 1) * 128], XNT[:, kc, tok0:tok0 + 512], kc == 0, kc == 7)
                    cp('dve', ZX[:, 3 + tci * 512:3 + tci * 512 + 512], PS[tci][:, :])
                    act(GY[:, tci * 512:tci * 512 + 512], PS[2 + tci][:, :], AF.Gelu_apprx_tanh)
                act(XC, ZX[:, 3:3 + TL], AF.Identity, bias=cb, scale=cw[3])
                for k in range(3):
                    stt(XC, ZX[:, k:k + TL], cw[k], XC, ALU.mult, ALU.add)
                cp('pool', XCB, XC)
                for tci in range(2):
                    mm(PS[4 + tci][:, :], WABD[:, ci, :], XCB[:, tci * 512:tci * 512 + 512], True, True)
                    mm(PS[6 + tci][:, :], WXBD[:, ci, :], XCB[:, tci * 512:tci * 512 + 512], True, True)
                for tci in range(2):
                    act(A_[:, tci * 512:tci * 512 + 512], PS[4 + tci][:, :], AF.Sigmoid, bias=ba)
                    act(I_[:, tci * 512:tci * 512 + 512], PS[6 + tci][:, :], AF.Sigmoid, bias=bx)
                act(A_, A_, AF.Exp, scale=CL[:, ci:ci + 1])
                tt('dve', T_, A_, A_, ALU.mult)
                act(T_, T_, AF.Sqrt, bias=1.0, scale=-1.0)
                tt('dve', I_, I_, XC, ALU.mult)
                tt('dve', I_, I_, T_, ALU.mult)
                init = 0.0 if hf == 0 else HLAST[:, 0:1]
                rd = [A_, I_] + ([] if hf == 0 else [HLAST])
                fw.op('dve', lambda e, init=init: e.tensor_tensor_scan(out=T_, data0=A_, data1=I_, initial=init, op0=ALU.mult, op1=ALU.add),
                      reads=rd, writes=[T_])
                cp('dve', HLAST, T_[:, TL - 1:TL])
                tt('dve', GY, T_, GY, ALU.mult)
                tt('pool', OSQ, GY, GY, ALU.mult)
                act(OLT[:, ci, hf * TL:(hf + 1) * TL], GY, AF.Identity, scale=gl)
                for tl in range(8):
                    mm(PS[0][:, tl:tl + 1], OSQ[:, tl * 128:(tl + 1) * 128], onesb[:, 0:1], tl == 0, tl == 7)
                if ci == 0:
                    cp('dve', SSL[:, hf * 8:hf * 8 + 8], PS[0][:, 0:8])
                else:
                    tt('dve', SSL[:, hf * 8:hf * 8 + 8], SSL[:, hf * 8:hf * 8 + 8], PS[0][:, 0:8], ALU.add)
        rstd_from_ss(SSL, RSL, 512)
        if stop == 'lru':
            dump('olt', OLT, BF16)
            dump('rsl', RSL)
            break
        fw.dma('sp', olt_d, OLT)
        ar.top = lru_top

        KCT = ar.alloc([2, 128], BF16)
        VCA = ar.alloc([2, 97], BF16)
        for g in range(2):
            cp('pool', VCA[:, g, 64:97], vcaux)
        cmp_top = ar.top
        WB = [ar.alloc([8, 512], BF16) for _ in range(1)]
        win_v = W['w_in'][l].rearrange("(kc p) n -> p kc n", p=128)

        def load_dup(wb, base_col):
            pass

        wb = WB[0]
        for j, c0 in enumerate([512, 576, 640, 704]):
            fw.dma('pool', wb[:, :, j * 128:j * 128 + 64], win_v[:, :, c0:c0 + 64])
            fw.dma('pool', wb[:, :, j * 128 + 64:j * 128 + 128], win_v[:, :, c0:c0 + 64])
        W1 = ar.alloc([2, 16, 256], BF16)
        for kv in range(2):
            fw.dma('pool', W1[:, kv, :, :], W['cmp_w1'][l, kv].rearrange("(i p) j -> p i j", p=128))
        W2K = ar.alloc([2, 128], BF16)
        W2V = ar.alloc([2, 64], BF16)
        fw.dma('pool', W2K[:, :, 0:64], W['cmp_w2'][l, 0].rearrange("(jc p) d -> p jc d", p=128))
        fw.dma('pool', W2K[:, :, 64:128], W['cmp_w2'][l, 0].rearrange("(jc p) d -> p jc d", p=128))
        fw.dma('pool', W2V, W['cmp_w2'][l, 1].rearrange("(jc p) d -> p jc d", p=128))
        B2V = ar.alloc([64], BF16)
        fw.dma('pool', B2V[0:1, :], W['cmp_b2'][l, 1:2, :])
        POSC = ar.alloc([2, 16], BF16)
        PB1 = ar.alloc([4], F32)
        for kv in range(2):
            fw.dma('sp', ROWS[64 + kv * 16:64 + kv * 16 + 16, :], W['cmp_pos'][l, kv].rearrange("(i a) d -> i (a d)", a=2))
        tr(PS[1][:, 0:32], ROWS[64:96, :], identf[64:96, 64:96])
        cp('act', POSC, PS[1][:, 0:32].rearrange("p (a b) -> p a b", a=2))
        for kv in range(2):
            for jc in range(2):
                for i in range(16):
                    mm(PS[1][:, 64 + kv * 2 + jc:65 + kv * 2 + jc], W1[:, kv, i, jc * 128:(jc + 1) * 128], POSC[:, kv, i:i + 1], i == 0, i == 15)
        tt('dve', PB1, PS[1][:, 64:68], COLA[:, 40:44], ALU.add)
        KC2 = ar.alloc([4, S], BF16)
        memset('pool', KC2[64:128, :, S - 1:S], 0.0)
        for j in range(4):
            for tc in range(4):
                bank = PS[2 + (j * 4 + tc) % 2]
                for kc in range(8):
                    mm(bank[:, :], wb[:, kc, j * 128:(j + 1) * 128], XNT[:, kc, tc * 512:(tc + 1) * 512], kc == 0, kc == 7)
                evac(KC2[0:64, j, tc * 512:(tc + 1) * 512], bank[0:64, :])
                if tc == 0:
                    evac(KC2[64:128, j, 0:511], bank[64:128, 1:512])
                else:
                    evac(KC2[64:128, j, tc * 512 - 1:tc * 512 + 511], bank[64:128, :])
        H1T = ar.alloc([2, 128], BF16)
        for kv in range(2):
            for g in range(2):
                j = kv * 2 + g
                for jc in range(2):
                    bank = PS[4 + jc]
                    for i in range(16):
                        rhs = KC2[:, j, 2 * i:2 * i + 16 * 126 + 1:16]
                        mm(bank[:, 0:127], W1[:, kv, i, jc * 128:(jc + 1) * 128], rhs, i == 0, i == 15)
                    act(H1T[:, jc, 0:127], bank[:, 0:127], AF.Gelu_apprx_tanh, bias=PB1[:, kv * 2 + jc:kv * 2 + jc + 1])
                if kv == 0:
                    bank = PS[6]
                    for jc in range(2):
                        mm(bank[:, 0:127], W2K[:, jc, :], H1T[:, jc, 0:127], jc == 0, jc == 1)
                    act(KCT[:, g, 0:127], bank[:, 0:127], AF.Identity, bias=COLA[:, 44:45])
                else:
                    bank = PS[7]
                    for jc in range(2):
                        mm(bank[0:127, 0:64], H1T[:, jc, 0:127], W2V[:, jc, :], jc == 0, False)
                    mm(bank[0:127, 0:64], onesb[0:1, 0:127], B2V[0:1, :], False, True)
                    cp('dve', VCA[0:127, g, 0:64], bank[0:127, 0:64])
        if stop == 'cmp':
            dump('kct', KCT, BF16)
            dump('vca', VCA, BF16)
            break
        ar.top = cmp_top
        QT = ar.alloc([4, S], BF16)
        KS = ar.alloc([2, S], BF16)
        KW = ar.alloc([2, S], BF16)
        VSf = ar.alloc([NT * 2 * 72], BF16)
        VWf = ar.alloc([NT * 2 * 72], BF16)
        memset('dve', VSf, 1.0)
        memset('dve', VWf, 1.0)
        VS = VSf.rearrange("p (t g d) -> p t g d", t=NT, g=2)
        VW = VWf.rearrange("p (t g d) -> p t g d", t=NT, g=2)
        proj_top = ar.top
        WB = [ar.alloc([8, 512], BF16) for _ in range(2)]
        wb = WB[1]
        fw.dma('pool', wb, win_v[:, :, 0:512])
        for pi in range(4):
            for tc in range(4):
                bank = PS[(pi * 4 + tc) % 2]
                for kc in range(8):
                    mm(bank[:, :], wb[:, kc, pi * 128:(pi + 1) * 128], XNT[:, kc, tc * 512:(tc + 1) * 512], kc == 0, kc == 7)
                evac(QT[:, pi, tc * 512:(tc + 1) * 512], bank[:, :])
        if stop == 'proj2':
            dump('qt', QT, BF16)
            break
        wb = WB[0]
        for j, c0 in enumerate([768, 832, 1024, 1088]):
            fw.dma('pool', wb[:, :, j * 128:j * 128 + 64], win_v[:, :, c0:c0 + 64])
            fw.dma('pool', wb[:, :, j * 128 + 64:j * 128 + 128], win_v[:, :, c0:c0 + 64])
        for j in range(4):
            dst = KS if j < 2 else KW
            for tc in range(4):
                bank = PS[2 + (j * 4 + tc) % 2]
                for kc in range(8):
                    mm(bank[:, :], wb[:, kc, j * 128:(j + 1) * 128], XNT[:, kc, tc * 512:(tc + 1) * 512], kc == 0, kc == 7)
                evac(dst[:, j % 2, tc * 512:(tc + 1) * 512], bank[:, :])
        if stop == 'proj3':
            dump('qt', QT, BF16)
            dump('ks', KS, BF16)
            break
        wb = WB[1]
        WT = ar.alloc([8, 320], BF16)
        fw.dma('pool', WT[:, :, 0:128], win_v[:, :, 896:1024])
        fw.dma('pool', WT[:, :, 128:256], win_v[:, :, 1152:1280])
        fw.dma('pool', WT[:, :, 256:320], win_v[:, :, 1280:1344])
        ZG = ar.alloc([24], F32)
        import os as _os
        for t in range(int(_os.environ.get('SUB4_NT', NT))):
            bank = PS[4 + t % 2]
            _m = int(_os.environ.get('SUB4_MODE', 9))
            if _m < 1:
                continue
            bks = [PS[2 + (t % 2) * 3 + i_] for i_ in range(3)]
            for gi_, (c0, c1) in enumerate([(0, 128), (128, 256), (256, 320)]):
                for kc in range(8):
                    mm(bks[gi_][:, 0:c1 - c0], XNT[:, kc, t * 128:(t + 1) * 128], WT[:, kc, c0:c1], kc == 0, kc == 7)
            if _m < 2:
                continue
            _ce = _os.environ.get('SUB4_ENG', 'dve')
            if _os.environ.get('SUB4_DST'):
                _tmpd = ar.alloc([64], BF16)
                for g in range(2):
                    cp(_ce, _tmpd, bank[:, g * 64:g * 64 + 64])
                    cp(_ce, _tmpd, bank[:, 128 + g * 64:128 + g * 64 + 64])
            else:
              for g in range(2):
                cp(_ce, VS[:, t, g, 0:64], bks[0][:, g * 64:g * 64 + 64])
                cp(_ce, VW[:, t, g, 0:64], bks[1][:, g * 64:g * 64 + 64])
            if _m < 3:
                continue
            tt('dve', ZG, bks[2][:, 0:24], GBB, ALU.add)
            if _m < 4:
                continue
            act(GATES[:, t, :], ZG, AF.Sigmoid)
        if stop == 'proj':
            dump('qt', QT, BF16)
            dump('gates', GATES)
            break
        ar.top = proj_top

        ar.top = xnt_off
        PT = [ar.alloc([512], BF16) for _ in range(3)]
        ON = ar.alloc([4, 512], F32)
        ONB = ar.alloc([4, 512], BF16)
        ONT = ar.alloc([4, S], BF16)
        assert ar.top <= xnt_off + 8 * S * 2
        ar.top = proj_top
        IMP = ar.alloc([4, 32], F32)
        TMPI = ar.alloc([4, 32], F32)
        TMPO = ar.alloc([4, 64], F32)
        DEN = ar.alloc([4], F32)
        CF = ar.alloc([4], F32)
        TOP8 = ar.alloc([8], F32)
        SEL = ar.alloc([4, 32], F32)
        NEGB = ar.alloc([4, 32], BF16)
        NEGBT = ar.alloc([512], BF16)
        junk2 = ar.alloc([512], BF16)
        memset('pool', NEGBT, 0.0)
        st_ctr = [0]
        acc_ctr = [0]

        def finish_branch(acc, qc, h, b, first):
            ts('dve', DEN, acc[:, :, 64], 1e-30, ALU.max)
            fw.op('dve', lambda e: e.reciprocal(out=DEN, in_=DEN), reads=[DEN], writes=[DEN])
            tt('dve', CF, DEN, GATES[:, 4 * qc:4 * qc + 4, 3 * h + b], ALU.mult)
            onv = ON[:, :, h * 64:(h + 1) * 64]
            if first:
                tt('dve', onv, acc[:, :, 0:64], CF.unsqueeze(2).to_broadcast([128, 4, 64]), ALU.mult)
            else:
                tt('dve', TMPO, acc[:, :, 0:64], CF.unsqueeze(2).to_broadcast([128, 4, 64]), ALU.mult)
                tt('pool', onv, onv, TMPO, ALU.add)

        for qc in range(4):
            for g in range(2):
                for r in range(4):
                    h = 4 * g + r
                    pi, half = h // 2, h % 2
                    p0 = half * 64
                    st = PS[st_ctr[0] % 3]
                    pt = PT[st_ctr[0] % 3]
                    st_ctr[0] += 1
                    mm(st[0:127, :], KCT[p0:p0 + 64, g, 0:127], QT[p0:p0 + 64, pi, qc * 512:(qc + 1) * 512], True, False)
                    mm(st[0:127, :], identb[0:127, 0:127], cmpb[0:127, qc * 512:(qc + 1) * 512], False, True)
                    act(pt[0:127, :], st[0:127, :], AF.Exp, scale=0.125)
                    accb = PS[3 + acc_ctr[0] % 2]
                    acc_ctr[0] += 1
                    acc = accb[:, 0:388].rearrange("p (i w) -> p i w", i=4)
                    for i in range(4):
                        mm(acc[:, i, :], pt[0:127, i * 128:(i + 1) * 128], VCA[0:127, g, :], i == 0, i == 3)
                    finish_branch(acc, qc, h, 0, True)
                    if r == 0:
                        tt('dve', IMP, acc[:, :, 65:97], DEN.unsqueeze(2).to_broadcast([128, 4, 32]), ALU.mult)
                    else:
                        tt('dve', TMPI, acc[:, :, 65:97], DEN.unsqueeze(2).to_broadcast([128, 4, 32]), ALU.mult)
                        tt('pool', IMP, IMP, TMPI, ALU.add)
                tt('dve', IMP, IMP, cbias[:, 4 * qc:4 * qc + 4, :], ALU.add)
                for i in range(4):
                    fw.op('dve', lambda e, i=i: e.max(out=TOP8, in_=IMP[:, i, :]), reads=[IMP[:, i, :]], writes=[TOP8])
                    ts('dve', SEL[:, i, :], IMP[:, i, :], TOP8[:, 7:8], ALU.is_ge)
                ts('dve', SEL, SEL, -1.0, ALU.add, -NEG, ALU.mult)
                tt('dve', NEGB, SEL, validb[:, 4 * qc:4 * qc + 4, :], ALU.add)
                if dbg and l == 0 and qc == 3 and g == 0:
                    dump('imp', IMP)
                    dump('negb', NEGB, BF16)
                items = []
                for r in range(4):
                    h = 4 * g + r
                    pi, half = h // 2, h % 2
                    p0 = half * 64
                    for b, KK, VV in ((1, KS, VS), (2, KW, VW)):
                        kb_lo = 0 if b == 1 else max(0, 4 * qc - 4)
                        kb_hi = 4 * qc + 3
                        accb = PS[3 + acc_ctr[0] % 2]
                        acc_ctr[0] += 1
                        acc = accb[:, 0:260].rearrange("p (i w) -> p i w", i=4)
                        for kb in range(kb_lo, kb_hi + 1):
                            qlo = max(kb, 4 * qc)
                            qhi = 4 * qc + 3 if b == 1 else min(kb + 4, 4 * qc + 3)
                            c0 = (qlo - 4 * qc) * 128
                            c1 = (qhi - 4 * qc + 1) * 128
                            bi = st_ctr[0] % 3
                            st_ctr[0] += 1
                            items.append(dict(st=PS[bi], pt=PT[bi], acc=acc, kb=kb, qlo=qlo, qhi=qhi, c0=c0, c1=c1, b=b,
                                              KK=KK, VV=VV, p0=p0, pi=pi, h=h, first=(kb == kb_lo), last=(kb == kb_hi)))

                def emit_S(it):
                    st, c0, c1, kb, p0, pi, b = it['st'], it['c0'], it['c1'], it['kb'], it['p0'], it['pi'], it['b']
                    mm(st[:, c0:c1], it['KK'][p0:p0 + 64, g, kb * 128:(kb + 1) * 128], QT[p0:p0 + 64, pi, qc * 512 + c0:qc * 512 + c1], True, False)
                    if b == 1:
                        mm(st[:, c0:c1], emat[0:32, kb * 128:(kb + 1) * 128], NEGBT[0:32, c0:c1], False, False)
                    if kb >= 4 * qc:
                        mm(st[:, c0:c0 + 128], identb, tri1, False, False)
                    if b == 2 and kb + 4 <= 4 * qc + 3:
                        mm(st[:, c1 - 128:c1], identb, tri2, False, False)

                def emit_E(it):
                    act(it['pt'][:, it['c0']:it['c1']], it['st'][:, it['c0']:it['c1']], AF.Exp, scale=0.125)

                def emit_P(it):
                    for qb in range(it['qlo'], it['qhi'] + 1):
                        i = qb - 4 * qc
                        mm(it['acc'][:, i, :], it['pt'][:, i * 128:(i + 1) * 128], it['VV'][:, it['kb'], g, 0:65],
                           it['first'] and qb == it['qlo'], False)
                    if it['last']:
                        finish_branch(it['acc'], qc, it['h'], it['b'], False)

                def run_pipe(its):
                    n_it = len(its)
                    for it in its:
                        it['st'] = PS[st_ctr[0] % 3]
                        it['pt'] = PT[st_ctr[0] % 3]
                        st_ctr[0] += 1
                    for k in range(min(2, n_it)):
                        emit_S(its[k])
                    for k in range(n_it):
                        emit_E(its[k])
                        emit_P(its[k])
                        if k + 2 < n_it:
                            emit_S(its[k + 2])

                run_pipe([it for it in items if it['b'] == 2])
                pbt = PSB[5][:, 0:512]
                for i in range(4):
                    tr(pbt[0:32, i * 128:(i + 1) * 128], NEGB[:, i, :], identb)
                cp('act', NEGBT[0:32, :], pbt[0:32, :])
                run_pipe([it for it in items if it['b'] == 1])
            for i in range(4):
                act(junk2, ON[:, i, :], AF.Square, accum=SSN[:, 4 * qc + i:4 * qc + i + 1])
            cp('pool', ONB, ON)
            for cc in range(4):
                pb = PSB[6 + cc % 2][:, 0:512]
                for i in range(4):
                    tr(pb[:, i * 128:(i + 1) * 128], ONB[:, i, cc * 128:(cc + 1) * 128], identb)
                act(ONT[:, cc, qc * 512:(qc + 1) * 512], pb, AF.Identity, scale=COLA[:, 36 + cc:37 + cc])
        rstd_from_ss(SSN, RSN, 512)
        if stop == 'attn':
            dump('ont', ONT, BF16)
            dump('rsn', RSN)
            dump('kct', KCT, BF16)
            dump('vca', VCA, BF16)
            dump('qt', QT, BF16)
            dump('gates', GATES)
            break

        ar.top = cmp_top
        OLT2 = ar.alloc([4, S], BF16)
        fw.dma('sp', OLT2, olt_d)
        WO = ar.alloc([8, D], BF16)
        fw.dma('pool', WO[:, 0:4, :], W['w_out'][l].rearrange("(kc p) n -> p kc n", p=128)[:, 0:4, :])
        fw.dma('pool', WO[:, 4:8, :], W['w_out'][l].rearrange("(kc p) n -> p kc n", p=128)[:, 4:8, :])
        for kc in range(8):
            tt('pool', WO[:, kc, :], WO[:, kc, :], MOD[:, 2, :], ALU.mult)
        for t in range(NT):
            for nh in range(2):
                b1 = PS[(t * 2 + nh) % 2]
                b2 = PS[2 + (t * 2 + nh) % 2]
                for cc in range(4):
                    mm(b1[:, :], ONT[:, cc, t * 128:(t + 1) * 128], WO[:, cc, nh * 512:(nh + 1) * 512], cc == 0, cc == 3)
                for cc in range(4):
                    mm(b2[:, :], OLT2[:, cc, t * 128:(t + 1) * 128], WO[:, 4 + cc, nh * 512:(nh + 1) * 512], cc == 0, cc == 3)
                xv = X[:, t, nh * 512:(nh + 1) * 512]
                stt(xv, b1[:, :], RSN[:, t:t + 1], xv, ALU.mult, ALU.add)
                stt(xv, b2[:, :], RSL[:, t:t + 1], xv, ALU.mult, ALU.add)
        if stop == 'mixer':
            dump('x1', X)
            break

        ar.top = mix_top
        MOD = ar.alloc([3, D], F32)
        XNT = ar.alloc([8, S], BF16)
        mark_ = ar.top
        GN = ar.alloc([D], F32)
        fw.dma('sp', GN, W['ffn_norm_g'][l].partition_broadcast(128))
        mod_half(l, 1, MOD, GN)
        ar.top = mark_
        norm_to_xnt(XNT, MOD[:, 1, :], MOD[:, 0, :])
        fw.dma('sp', ROWS[0:66, :], W['ffn_conv_w'][l].rearrange("k (m p) -> (k m) p", p=128))
        fw.dma('sp', ROWS[66:88, :], W['ffn_conv_b'][l].rearrange("(m p) -> m p", p=128))
        tr(PS[0][:, 0:88], ROWS[0:88, :], identf[0:88, 0:88])
        cp('act', COLB[:, 0:88], PS[0][:, 0:88])
        HMT = ar.alloc([5, S], BF16)
        WD = [ar.alloc([5, D], BF16) for _ in range(2)]
        WGU = [ar.alloc([2, 8, 256], BF16) for _ in range(2)]
        GRAW = ar.alloc([2 + S], F32)
        CT = [ar.alloc([512], F32) for _ in range(2)]
        SG = [ar.alloc([512], F32) for _ in range(2)]
        memset('dve', GRAW[:, 0:2], 0.0)
        wg_v = W['ffn_w_gate'][l].rearrange("(kc p) n -> p kc n", p=128)
        wu_v = W['ffn_w_up'][l].rearrange("(kc p) n -> p kc n", p=128)
        wd_v = W['ffn_w_down'][l].rearrange("(m p) n -> p m n", p=128)
        slab_ctr = [0]

        def load_slab(m0, n):
            sl = WGU[slab_ctr[0] % 2]
            slab_ctr[0] += 1
            fw.dma('pool', sl[:, 0, :, 0:n * 128], wg_v[:, :, m0 * 128:(m0 + n) * 128])
            fw.dma('pool', sl[:, 1, :, 0:n * 128], wu_v[:, :, m0 * 128:(m0 + n) * 128])
            return sl

        slabs = [(m0, min(2, NM - m0)) for m0 in range(0, NM, 2)]
        slab_bufs = {}
        slab_bufs[0] = load_slab(*slabs[0])
        ck = 0
        for gi, (gm0, gn) in enumerate(FFN_GROUPS):
            wd = WD[gi % 2]
            fw.dma('pool', wd[:, 0:gn, :], wd_v[:, gm0:gm0 + gn, :])
            for mloc in range(gn):
                tt('pool', wd[:, mloc, :], wd[:, mloc, :], MOD[:, 2, :], ALU.mult)
            for mloc in range(gn):
                m = gm0 + mloc
                si = m // 2
                if m % 2 == 0 and si + 1 < len(slabs):
                    slab_bufs[si + 1] = load_slab(*slabs[si + 1])
                sl = slab_bufs[si]
                sc = (m % 2) * 128
                w0 = COLB[:, 0 * NM + m:0 * NM + m + 1]
                w1 = COLB[:, 1 * NM + m:1 * NM + m + 1]
                w2 = COLB[:, 2 * NM + m:2 * NM + m + 1]
                cb = COLB[:, 66 + m:67 + m]
                for tc in range(4):
                    bg = PS[(ck % 2) * 2]
                    bu = PS[(ck % 2) * 2 + 1]
                    ct = CT[ck % 2]
                    sg = SG[ck % 2]
                    ck += 1
                    for kc in range(8):
                        mm(bg[:, :], sl[:, 0, kc, sc:sc + 128], XNT[:, kc, tc * 512:(tc + 1) * 512], kc == 0, kc == 7)
                    for kc in range(8):
                        mm(bu[:, :], sl[:, 1, kc, sc:sc + 128], XNT[:, kc, tc * 512:(tc + 1) * 512], kc == 0, kc == 7)
                    if tc == 0:
                        pass
                    cp('act', GRAW[:, 2 + tc * 512:2 + (tc + 1) * 512], bg[:, :])
                    act(ct, bg[:, :], AF.Identity, bias=cb, scale=w2)
                    stt(ct, GRAW[:, tc * 512 + 1:tc * 512 + 513], w1, ct, ALU.mult, ALU.add)
                    stt(ct, GRAW[:, tc * 512:tc * 512 + 512], w0, ct, ALU.mult, ALU.add)
                    act(sg, ct, AF.Silu)
                    tt('dve', HMT[:, mloc, tc * 512:(tc + 1) * 512], sg, bu[:, :], ALU.mult)
            for t in range(NT):
                for nh in range(2):
                    bd = PS[4 + (t * 2 + nh) % 3]
                    for mloc in range(gn):
                        mm(bd[:, :], HMT[:, mloc, t * 128:(t + 1) * 128], wd[:, mloc, nh * 512:(nh + 1) * 512], mloc == 0, mloc == gn - 1)
                    xv = X[:, t, nh * 512:(nh + 1) * 512]
                    tt('dve', xv, bd[:, :], xv, ALU.add)
        if stop == 'ffn':
            dump('x2', X)
            break

    if stop is None:
        ar.top = setup_top
        GF = ar.alloc([D], F32)
        fw.dma('sp', GF, W['final_norm_g'].partition_broadcast(128))
        junk = ar.alloc([D], BF16)
        OB = [ar.alloc([D], F32) for _ in range(2)]
        for t in range(NT):
            act(junk, X[:, t, :], AF.Square, accum=SS[:, t:t + 1])
        rstd_from_ss(SS, RS, D)
        for t in range(NT):
            ob = OB[t % 2]
            stt(ob, X[:, t, :], RS[:, t:t + 1], GF, ALU.mult, ALU.mult)
            fw.dma('sp', out_d[t * 128:(t + 1) * 128, :], ob, is_output=True)
    fw.finish('sp')
    fw.emit()
    nc._dbg_outs = dbg_outs
    nc._fw = fw
    nc._arena_peak = ar.peak
    return nc


_CACHE = {}


def make_in_maps(inputs, n_layers=L):
    consts = make_consts()
    x = np.ascontiguousarray(np.asarray(inputs['x'], dtype=np.float32))
    c = np.ascontiguousarray(np.asarray(inputs['c'], dtype=np.float32))
    shared = {}
    for nm, shp in WEIGHT_SPECS:
        a = np.asarray(inputs[nm], dtype=np.float32)
        if nm != 'final_norm_g':
            a = a[0:n_layers]
        shared[nm] = np.ascontiguousarray(a)
    shared.update(consts)
    in_maps = []
    for b in range(8):
        m = dict(shared)
        m['x'] = x[b]
        m['c'] = c[b].reshape(8, 128)
        in_maps.append(m)
    return in_maps


def kernel(**inputs):
    if 'nc' not in _CACHE:
        _CACHE['nc'] = build_program()
    nc = _CACHE['nc']
    in_maps = make_in_maps(inputs)
    res = run_bass_kernel_spmd(nc, in_maps, core_ids=list(range(8)))
    out = np.stack([np.asarray(r['out'], dtype=np.float32) for r in res.results], axis=0)
    return out
```
